# Optimizing a Trainium2 kernel written in Bass

```python
import jax, jax.numpy as jnp
from jax import lax
import numpy as np

D_MODEL = 1024
BATCH = 4
SEQ = 8192
DEPTH = 4

FOURIER_WIDTH = D_MODEL // 2
FOURIER_GROUPS = 4
CONV_WIDTH = D_MODEL // 2
CONV_KERNEL = 31
HEAD_DIM = 64
N_Q_HEADS = D_MODEL // HEAD_DIM
N_KV_HEADS = N_Q_HEADS // 4
WINDOW = 128
BLOCK = 128
ROPE_THETA = 10000.0
N_EXPERTS = 16
EXPERT_FF = D_MODEL
CAPACITY_FACTOR = 2
EPS = 1e-6
N_EVEN = (DEPTH + 1) // 2
N_ODD = DEPTH // 2

kernel_name = "hybrid_fnet_conformer_swa_ecmoe_encoder"

F32 = jnp.float32


def rms_norm(x, g):
    xf = x.astype(F32)
    y = xf * lax.rsqrt(jnp.mean(xf * xf, axis=-1, keepdims=True) + EPS)
    return (y * g.astype(F32)).astype(x.dtype)


def layer_norm(x, g, b):
    xf = x.astype(F32)
    mu = jnp.mean(xf, axis=-1, keepdims=True)
    var = jnp.mean(jnp.square(xf - mu), axis=-1, keepdims=True)
    y = (xf - mu) * lax.rsqrt(var + EPS)
    return (y * g.astype(F32) + b.astype(F32)).astype(x.dtype)


def adaln(cond, w, b):
    mod = cond @ w + b
    shift, scale, gate = jnp.split(mod, 3, axis=-1)
    return shift[:, None, :], scale[:, None, :], gate[:, None, :]


def rope_tables(positions):
    inv = ROPE_THETA ** (-jnp.arange(0, HEAD_DIM, 2, dtype=F32) / HEAD_DIM)
    ang = positions.astype(F32)[..., None] * inv
    return jnp.cos(ang), jnp.sin(ang)


def apply_rope(t, cos, sin):
    half = HEAD_DIM // 2
    t1, t2 = t[..., :half].astype(F32), t[..., half:].astype(F32)
    c, s = cos[:, :, None, :], sin[:, :, None, :]
    return jnp.concatenate([t1 * c - t2 * s, t2 * c + t1 * s], axis=-1).astype(t.dtype)


def fourier_conv_mixer(h, w_in, conv_w, conv_b, ln_g, ln_b, w_out):
    B, S, _ = h.shape
    proj = h @ w_in
    u_f = proj[..., :FOURIER_WIDTH]
    u_v = proj[..., FOURIER_WIDTH:FOURIER_WIDTH + CONV_WIDTH]
    u_g = proj[..., FOURIER_WIDTH + CONV_WIDTH:]
    uf = u_f.astype(F32).reshape(B, S, FOURIER_GROUPS, FOURIER_WIDTH // FOURIER_GROUPS)
    y_f = jnp.fft.fft2(uf, axes=(1, 3), norm="ortho").real.reshape(B, S, FOURIER_WIDTH).astype(h.dtype)
    glu = u_v * jax.nn.sigmoid(u_g)
    pad = CONV_KERNEL // 2
    conv = lax.conv_general_dilated(
        glu, conv_w[:, None, :], window_strides=(1,), padding=[(pad, pad)],
        dimension_numbers=("NWC", "WIO", "NWC"), feature_group_count=CONV_WIDTH) + conv_b
    y_c = jax.nn.silu(layer_norm(conv, ln_g, ln_b))
    return jnp.concatenate([y_f, y_c], axis=-1) @ w_out


def windowed_attention(h, w_qkv, sink, w_out, cos, sin):
    B, S, _ = h.shape
    nb = S // BLOCK
    G = N_Q_HEADS // N_KV_HEADS
    qd, kd = N_Q_HEADS * HEAD_DIM, N_KV_HEADS * HEAD_DIM
    qkv = h @ w_qkv
    q = apply_rope(qkv[..., :qd].reshape(B, S, N_Q_HEADS, HEAD_DIM), cos, sin)
    k = apply_rope(qkv[..., qd:qd + kd].reshape(B, S, N_KV_HEADS, HEAD_DIM), cos, sin)
    v = qkv[..., qd + kd:].reshape(B, S, N_KV_HEADS, HEAD_DIM)
    qb = q.reshape(B, nb, BLOCK, N_KV_HEADS, G, HEAD_DIM)

    def band(t):
        tp = jnp.pad(t, ((0, 0), (BLOCK, BLOCK), (0, 0), (0, 0))).reshape(B, nb + 2, BLOCK, N_KV_HEADS, HEAD_DIM)
        return jnp.concatenate([tp[:, :-2], tp[:, 1:-1], tp[:, 2:]], axis=2)

    kb, vb = band(k), band(v)
    s = jnp.einsum("bnqkgd,bnjkd->bnkgqj", qb, kb, preferred_element_type=F32) * (HEAD_DIM ** -0.5)
    qi = jnp.arange(BLOCK)[:, None]
    kj = jnp.arange(3 * BLOCK)[None, :]
    rel = kj - BLOCK - qi
    kpos = (jnp.arange(nb)[:, None, None] - 1) * BLOCK + kj[None]
    valid = (jnp.abs(rel) <= WINDOW)[None] & (kpos >= 0) & (kpos < S)
    s = jnp.where(valid[None, :, None, None], s, -jnp.inf)
    sink_l = sink.astype(F32).reshape(N_KV_HEADS, G)[None, None, :, :, None, None]
    m = jnp.maximum(jnp.max(s, axis=-1, keepdims=True), sink_l)
    e = jnp.exp(s - m)
    p = e / (jnp.sum(e, axis=-1, keepdims=True) + jnp.exp(sink_l - m))
    o = jnp.einsum("bnkgqj,bnjkd->bnqkgd", p.astype(h.dtype), vb).reshape(B, S, qd)
    return o @ w_out


def ec_moe(h, router_w, router_b, w_gate, w_up, w_down):
    B, S, _ = h.shape
    cap = CAPACITY_FACTOR * S // N_EXPERTS
    logits = jnp.einsum("bsd,de->bse", h, router_w, preferred_element_type=F32) + router_b.astype(F32)
    aff = jax.nn.softmax(logits, axis=-1)
    gate, idx = lax.top_k(jnp.swapaxes(aff, 1, 2), cap)
    bidx = jnp.arange(B)[:, None, None]
    xg = h[bidx, idx]
    hid = jax.nn.silu(jnp.einsum("becd,edf->becf", xg, w_gate)) * jnp.einsum("becd,edf->becf", xg, w_up)
    y = jnp.einsum("becf,efd->becd", hid, w_down) * gate[..., None].astype(h.dtype)
    return jnp.zeros_like(h).at[bidx, idx].add(y)


def setup_inputs(seed: int = 0) -> dict:
    key = jax.random.key(seed)
    ks = jax.random.split(key, 24)
    D, E, F = D_MODEL, N_EXPERTS, EXPERT_FF
    n = lambda k, shape, sc: jax.random.normal(k, shape, F32) * sc
    fw_in = FOURIER_WIDTH + 2 * CONV_WIDTH
    mix_out = FOURIER_WIDTH + CONV_WIDTH
    qkv_dim = (N_Q_HEADS + 2 * N_KV_HEADS) * HEAD_DIM
    offset = jax.random.randint(ks[2], (BATCH, 1), 0, 1024, dtype=jnp.int32)
    positions = offset + jnp.arange(SEQ, dtype=jnp.int32)[None, :]
    return {
        "x": n(ks[0], (BATCH, SEQ, D), 1.0),
        "c": n(ks[1], (BATCH, D), 1.0),
        "positions": positions,
        "ada_w": n(ks[3], (DEPTH, 2, D, 3 * D), 0.5 * D ** -0.5),
        "ada_b": n(ks[4], (DEPTH, 2, 3 * D), 0.02),
        "mix_norm_g": 1.0 + n(ks[5], (DEPTH, D), 0.02),
        "ffn_norm_g": 1.0 + n(ks[6], (DEPTH, D), 0.02),
        "fc_w_in": n(ks[7], (N_EVEN, D, fw_in), D ** -0.5),
        "conv_w": n(ks[8], (N_EVEN, CONV_KERNEL, CONV_WIDTH), CONV_KERNEL ** -0.5),
        "conv_b": n(ks[9], (N_EVEN, CONV_WIDTH), 0.02),
        "conv_ln_g": 1.0 + n(ks[10], (N_EVEN, CONV_WIDTH), 0.02),
        "conv_ln_b": n(ks[11], (N_EVEN, CONV_WIDTH), 0.02),
        "fc_w_out": n(ks[12], (N_EVEN, mix_out, D), mix_out ** -0.5),
        "attn_w_qkv": n(ks[13], (N_ODD, D, qkv_dim), D ** -0.5),
        "attn_sink": n(ks[14], (N_ODD, N_Q_HEADS), 1.0),
        "attn_w_out": n(ks[15], (N_ODD, N_Q_HEADS * HEAD_DIM, D), (N_Q_HEADS * HEAD_DIM) ** -0.5),
        "router_w": n(ks[16], (DEPTH, D, E), D ** -0.5),
        "router_b": n(ks[17], (DEPTH, E), 0.01),
        "moe_w_gate": n(ks[18], (DEPTH, E, D, F), D ** -0.5),
        "moe_w_up": n(ks[19], (DEPTH, E, D, F), D ** -0.5),
        "moe_w_down": n(ks[20], (DEPTH, E, F, D), F ** -0.5),
        "final_norm_g": 1.0 + n(ks[21], (D,), 0.02),
    }


def reference(x, c, positions, ada_w, ada_b, mix_norm_g, ffn_norm_g, fc_w_in, conv_w, conv_b,
              conv_ln_g, conv_ln_b, fc_w_out, attn_w_qkv, attn_sink, attn_w_out, router_w,
              router_b, moe_w_gate, moe_w_up, moe_w_down, final_norm_g):
    cos, sin = rope_tables(positions)
    cond = jax.nn.silu(c)
    for l in range(DEPTH):
        i = l // 2
        shift, scale, gate = adaln(cond, ada_w[l, 0], ada_b[l, 0])
        h = rms_norm(x, mix_norm_g[l]) * (1 + scale) + shift
        if l % 2 == 0:
            y = fourier_conv_mixer(h, fc_w_in[i], conv_w[i], conv_b[i], conv_ln_g[i], conv_ln_b[i], fc_w_out[i])
        else:
            y = windowed_attention(h, attn_w_qkv[i], attn_sink[i], attn_w_out[i], cos, sin)
        x = x + gate * y
        shift, scale, gate = adaln(cond, ada_w[l, 1], ada_b[l, 1])
        h = rms_norm(x, ffn_norm_g[l]) * (1 + scale) + shift
        x = x + gate * ec_moe(h, router_w[l], router_b[l], moe_w_gate[l], moe_w_up[l], moe_w_down[l])
    return rms_norm(x, final_norm_g)
```

```python
from contextlib import ExitStack
import numpy as np
import concourse.bass as bass
import concourse.mybir as mybir
from concourse.bass_utils import run_bass_kernel_spmd

F32 = mybir.dt.float32
BF16 = mybir.dt.bfloat16
I32 = mybir.dt.int32
AF = mybir.ActivationFunctionType
ALU = mybir.AluOpType
AX = mybir.AxisListType

PE, ACT, DVE, POOL, SP = "tensor", "scalar", "vector", "gpsimd", "sync"

D = 1024
SEQ = 8192
OWN = 4096
NE = 16
CAP = 1024
EPS = 1e-6


class _Op:
    __slots__ = ("eng", "fn", "deps", "dma", "sem_key", "idx", "signal", "val", "ninc")

    def __init__(self, eng, fn, dma, sem_key, idx):
        self.eng = eng
        self.fn = fn
        self.deps = set()
        self.dma = dma
        self.sem_key = sem_key
        self.idx = idx
        self.signal = dma
        self.val = None
        self.ninc = 1


class Sched:
    def __init__(self, nc):
        self.nc = nc
        self.ops = []
        self.lastw = {}
        self.readers = {}

    def add(self, eng, fn, reads=(), writes=(), dma=False, sem_key=None, ninc=1):
        idx = len(self.ops)
        if dma and sem_key is None:
            sem_key = writes[0]
        op = _Op(eng, fn, dma, sem_key, idx)
        op.ninc = ninc
        for k in reads:
            w = self.lastw.get(k)
            if w is not None:
                op.deps.add(w)
            self.readers.setdefault(k, []).append(idx)
        for k in writes:
            w = self.lastw.get(k)
            if w is not None:
                op.deps.add(w)
            for r in self.readers.get(k, ()):
                op.deps.add(r)
            self.readers[k] = []
            self.lastw[k] = idx
        op.deps.discard(idx)
        self.ops.append(op)
        return idx

    def pe(self, fn, reads=(), writes=()):
        return self.add(PE, fn, reads, writes)

    def act(self, fn, reads=(), writes=()):
        return self.add(ACT, fn, reads, writes)

    def dve(self, fn, reads=(), writes=()):
        return self.add(DVE, fn, reads, writes)

    def pool(self, fn, reads=(), writes=()):
        return self.add(POOL, fn, reads, writes)

    def dma(self, eng, fn, reads=(), writes=(), sem_key=None, ninc=1):
        return self.add(eng, fn, reads, writes, dma=True, sem_key=sem_key, ninc=ninc)

    def barrier(self):
        last = {}
        for op in self.ops:
            if op.fn is None:
                continue
            last[(op.sem_key if op.dma else op.eng, op.dma)] = op.idx
        for eng in (PE, ACT, DVE, POOL, SP):
            idx = len(self.ops)
            op = _Op(eng, None, False, None, idx)
            op.deps = set(last.values())
            self.ops.append(op)

    def emit(self):
        nc = self.nc
        ops = self.ops
        for op in ops:
            for d in op.deps:
                dop = ops[d]
                if dop.eng == PE and op.eng == PE and not dop.dma and not op.dma and op.fn is not None:
                    continue
                dop.signal = True
        EPOCH = 1500
        eng_cnt = {}
        eng_tot = {}
        dma_cnt = {}
        for op in ops:
            if op.dma:
                c = dma_cnt.get(op.sem_key, 0) + 16 * op.ninc
                dma_cnt[op.sem_key] = c
                op.val = c
            elif op.signal:
                t = eng_tot.get(op.eng, 0)
                ep = t // EPOCH
                eng_tot[op.eng] = t + 1
                c = eng_cnt.get((op.eng, ep), 0) + 1
                eng_cnt[(op.eng, ep)] = c
                op.val = c
                op.sem_key = (op.eng, ep)
        with ExitStack() as es:
            eng_sem = {k: es.enter_context(nc.semaphore("c_%s_%d" % k)) for k in eng_cnt}
            dma_sem = {}
            for i, k in enumerate(dma_cnt):
                dma_sem[k] = es.enter_context(nc.semaphore("d%d" % i))
            block = es.enter_context(nc.Block())
            per_eng = {e: [] for e in (PE, ACT, DVE, POOL, SP)}
            for op in ops:
                per_eng[op.eng].append(op)

            def sem_of(op):
                return dma_sem[op.sem_key] if op.dma else eng_sem[op.sem_key]

            def run_engine(ename, e):
                known = {}
                for op in per_eng[ename]:
                    need = {}
                    for d in op.deps:
                        dop = ops[d]
                        if dop.eng == PE and ename == PE and not dop.dma and not op.dma and op.fn is not None:
                            continue
                        s = sem_of(dop)
                        sid = id(s)
                        if need.get(sid, (None, -1))[1] < dop.val:
                            need[sid] = (s, dop.val)
                    for sid, (s, v) in need.items():
                        if known.get(sid, -1) >= v:
                            continue
                        e.wait_ge(s, v)
                        known[sid] = v
                    if op.fn is None:
                        continue
                    r = op.fn(e)
                    if op.dma:
                        if not isinstance(r, (list, tuple)):
                            r = [r]
                        assert len(r) == op.ninc, (len(r), op.ninc)
                        for ins in r:
                            ins.then_inc(dma_sem[op.sem_key], 16)
                    elif op.signal:
                        r.then_inc(eng_sem[op.sem_key], 1)
                for k, c in dma_cnt.items():
                    e.wait_ge(dma_sem[k], c)
                for k, c in eng_cnt.items():
                    e.wait_ge(eng_sem[k], c)

            @block.tensor
            def _(e):
                run_engine(PE, e)

            @block.scalar
            def _(e):
                run_engine(ACT, e)

            @block.vector
            def _(e):
                run_engine(DVE, e)

            @block.gpsimd
            def _(e):
                run_engine(POOL, e)

            @block.sync
            def _(e):
                run_engine(SP, e)


class Ctx:
    def __init__(self, nc, es):
        self.nc = nc
        self.es = es
        self.S = Sched(nc)
        self.ps = [es.enter_context(nc.psum_tensor("ps%d" % i, [128, 512], F32)) for i in range(8)]

    SB_LO = 16512
    SB_HI = 229344

    def sb(self, name, shape, dt):
        nbytes = int(np.prod(shape[1:])) * (2 if dt == BF16 else 4)
        off = getattr(self, "_off", self.SB_LO)
        off = (off + 31) // 32 * 32
        assert off + nbytes <= self.SB_HI, ("SBUF overflow", name, off, nbytes)
        self._off = off + nbytes
        self._n = getattr(self, "_n", 0) + 1
        return self.nc.alloc_sbuf_tensor_at("s%d_%s" % (self._n, name), list(shape), dt, offset=off)

    def mark(self):
        return getattr(self, "_off", self.SB_LO)

    def reset(self, m):
        self._off = m

    def dram_in(self, name, shape, dt):
        return self.nc.dram_tensor(name, list(shape), dt, kind="ExternalInput").ap()

    def dram_out(self, name, shape, dt):
        return self.nc.dram_tensor(name, list(shape), dt, kind="ExternalOutput").ap()


def emit_consts(C):
    S = C.S
    C.onesm = C.sb("onesm", [128, 128], BF16)
    C.epst = C.sb("epst", [128, 1], F32)
    S.dve(lambda e: e.memset(C.onesm[:], 1.0 / D), writes=["onesm"])
    S.dve(lambda e: e.memset(C.epst[:], EPS), writes=["epst"])


def build_ada():
    nc = bass.Bass("TRN2", target_bir_lowering=False)
    with ExitStack() as es:
        C = Ctx(nc, es)
        S = C.S
        c_in = C.dram_in("c", [4, D], F32)
        adaw = C.dram_in("adaw", [D, 3 * D], F32)
        adab = C.dram_in("adab", [3 * D], F32)
        out = C.dram_out("mod", [128, 24, 4], F32)
        cond = C.sb("cond", [128, 8, 4], F32)
        adb = C.sb("adb", [128, 24], F32)
        mod = C.sb("mod", [128, 24, 4], F32)
        wch = [C.sb("adaw%d" % i, [128, 8, 128], F32) for i in range(2)]
        for b in range(4):
            S.dma(SP, lambda e, b=b: e.dma_start(out=cond[:, :, b], in_=c_in[b].rearrange("(k p) -> p k", p=128),
                                                 allow_slow_non_contiguous=True), writes=[("cond", b)])
        S.dma(SP, lambda e: e.dma_start(out=adb[:], in_=adab.rearrange("(k p) -> p k", p=128),
                                        allow_slow_non_contiguous=True), writes=["adb"])
        S.act(lambda e: e.activation(out=cond[:], in_=cond[:], func=AF.Silu),
              reads=[("cond", b) for b in range(4)], writes=["condS"])
        ps = C.ps[0]
        for n in range(24):
            sl = n % 2
            S.dma(SP, lambda e, n=n, sl=sl: e.dma_start(
                out=wch[sl][:], in_=adaw[:, n * 128:(n + 1) * 128].rearrange("(k p) n -> p k n", p=128)),
                writes=[("adaw", sl)])

            def mm(e, n=n, sl=sl):
                for k in range(8):
                    r = e.matmul(ps[:, n * 4:(n + 1) * 4], lhsT=wch[sl][:, k, :], rhs=cond[:, k, :],
                                 start=(k == 0), stop=(k == 7))
                return r
            S.pe(mm, reads=[("adaw", sl), "condS"], writes=[("ps", 0)])
        for b in range(4):
            S.dve(lambda e, b=b: e.tensor_tensor(
                out=mod[:, :, b], in0=ps[:, 0:96].rearrange("p (n b) -> p n b", b=4)[:, :, b], in1=adb[:], op=ALU.add),
                reads=[("ps", 0), "adb"], writes=[("mod", b)])
        S.dma(SP, lambda e: e.dma_start(out=out, in_=mod[:]), reads=[("mod", b) for b in range(4)], writes=["OUT"])
        S.emit()
    return nc


def emit_adaln(C, mod_in, g_in):
    S = C.S
    gt = C.sb("gT", [128, 8], F32)
    C.mod = C.sb("mod", [128, 24], F32)
    C.amul = C.sb("amul", [128, 8], F32)
    S.dma(SP, lambda e: e.dma_start(out=gt[:], in_=g_in.rearrange("(k p) -> p k", p=128),
                                    allow_slow_non_contiguous=True), writes=["gT"])
    S.dma(SP, lambda e: e.dma_start(out=C.mod[:], in_=mod_in), writes=["mod"])
    S.dve(lambda e: e.scalar_tensor_tensor(out=C.amul[:], in0=C.mod[:, 8:16], scalar=1.0, in1=gt[:],
                                           op0=ALU.add, op1=ALU.mult), reads=["mod", "gT"], writes=["amul"])


def emit_norm(C, xs, xkey, out, okeys, T, psb, bufs, amul=None, akey="amul", shift="mod", tag="", xkeys=None):
    S = C.S
    amul = C.amul if amul is None else amul
    sq, rstd, tmp = bufs["sq"], bufs["rstd"], bufs["tmp"]
    ps = C.ps[psb]
    xk = [xkey] if xkeys is None else list(xkeys)
    S.act(lambda e: e.activation(out=sq[:, :, :T], in_=xs[:, :, :T], func=AF.Square),
          reads=xk, writes=["sq" + tag])

    def mm(e):
        for k in range(8):
            r = e.matmul(ps[:, :T], lhsT=C.onesm[:], rhs=sq[:, k, :T], start=(k == 0), stop=(k == 7))
        return r
    S.pe(mm, reads=["onesm", "sq" + tag], writes=[("ps", psb)])
    S.act(lambda e: e.activation(out=rstd[:, :T], in_=ps[:, :T], func=AF.Sqrt, bias=C.epst[:, 0:1], scale=1.0),
          reads=[("ps", psb), "epst"], writes=["rstd" + tag])
    S.dve(lambda e: e.reciprocal(out=rstd[:, :T], in_=rstd[:, :T]), reads=["rstd" + tag], writes=["rstd" + tag])
    for k in range(8):
        eng = DVE
        if shift is None:
            S.add(eng, lambda e, k=k: e.scalar_tensor_tensor(
                out=out[:, k, :T], in0=xs[:, k, :T], scalar=amul[:, k:k + 1], in1=rstd[:, :T],
                op0=ALU.mult, op1=ALU.mult), reads=xk + ["rstd" + tag, akey], writes=[okeys[k]])
        else:
            S.add(eng, lambda e, k=k: e.scalar_tensor_tensor(
                out=tmp[:, k, :T], in0=xs[:, k, :T], scalar=amul[:, k:k + 1], in1=rstd[:, :T],
                op0=ALU.mult, op1=ALU.mult), reads=xk + ["rstd" + tag, akey], writes=[("tmp" + tag, k)])
            S.act(lambda e, k=k: e.activation(out=out[:, k, :T], in_=tmp[:, k, :T], func=AF.Identity,
                                              bias=C.mod[:, k:k + 1], scale=1.0),
                  reads=[("tmp" + tag, k), "mod"], writes=[okeys[k]])


NITER = 24


def build_route():
    nc = bass.Bass("TRN2", target_bir_lowering=False)
    with ExitStack() as es:
        C = Ctx(nc, es)
        S = C.S
        xT = C.dram_in("xT", [D, SEQ], F32)
        mod_in = C.dram_in("modin", [128, 24], F32)
        g_in = C.dram_in("g", [D], F32)
        rw_in = C.dram_in("rw", [D, NE], F32)
        rb_in = C.dram_in("rb", [NE], F32)
        blk_in = C.dram_in("blkones", [128, 128], F32)
        sel_in = C.dram_in("sel", [128, NE], F32)
        out = C.dram_out("wgt", [NE, OWN], F32)
        affd = nc.dram_tensor("affd", [NE, SEQ], F32, kind="Internal").ap()

        emit_consts(C)
        emit_adaln(C, mod_in, g_in)

        T = 512
        NB = SEQ // T
        xs = [C.sb("xs%d" % i, [128, 8, T], F32) for i in range(2)]
        nb = {"sq": C.sb("sq", [128, 8, T], BF16), "rstd": C.sb("rstd", [128, T], F32),
              "tmp": C.sb("tmp", [128, 8, T], F32)}
        hf = C.sb("hf", [128, 8, T], F32)
        rw = C.sb("rw", [128, 8, NE], F32)
        rb = C.sb("rb", [NE, 1], F32)
        ones16 = C.sb("ones16", [NE, NE], F32)
        Eexp = C.sb("Eexp", [NE, T], F32)
        rcp = C.sb("rcp", [NE, T], F32)
        aff = C.sb("aff", [NE, SEQ], F32)
        S.dma(SP, lambda e: e.dma_start(out=rw[:], in_=rw_in.rearrange("(k p) n -> p k n", p=128)), writes=["rw"])
        S.dma(SP, lambda e: e.dma_start(out=rb[:], in_=rb_in.rearrange("(p o) -> p o", o=1)), writes=["rb"])
        S.dve(lambda e: e.memset(ones16[:], 1.0), writes=["ones16"])

        xv = xT.rearrange("(k p) t -> p k t", p=128)
        for j in range(NB):
            sl = j % 2
            S.dma(SP, lambda e, j=j, sl=sl: e.dma_start(out=xs[sl][:], in_=xv[:, :, j * T:(j + 1) * T]),
                  writes=[("xs", sl)])
            emit_norm(C, xs[sl], ("xs", sl), hf, [("hf", k) for k in range(8)], T, 0, nb)

            def mm(e):
                for k in range(8):
                    r = e.matmul(C.ps[1][0:NE, :], lhsT=rw[:, k, :], rhs=hf[:, k, :], start=(k == 0), stop=(k == 7))
                return r
            S.pe(mm, reads=["rw"] + [("hf", k) for k in range(8)], writes=[("ps", 1)])
            S.act(lambda e: e.activation(out=Eexp[:], in_=C.ps[1][0:NE, :], func=AF.Exp, bias=rb[:, 0:1], scale=1.0),
                  reads=[("ps", 1), "rb"], writes=["Eexp"])
            S.pe(lambda e: e.matmul(C.ps[2][0:NE, :], lhsT=ones16[:], rhs=Eexp[:], start=True, stop=True),
                 reads=["ones16", "Eexp"], writes=[("ps", 2)])
            S.dve(lambda e: e.reciprocal(out=rcp[:], in_=C.ps[2][0:NE, :]), reads=[("ps", 2)], writes=["rcp"])
            S.dve(lambda e, j=j: e.tensor_tensor(out=aff[:, j * T:(j + 1) * T], in0=Eexp[:], in1=rcp[:], op=ALU.mult),
                  reads=["Eexp", "rcp"], writes=["aff"])

        affF = C.sb("affF", [128, SEQ // 8], F32)
        mk = C.sb("mk", [128, SEQ // 8], F32)
        blk = C.sb("blk", [128, 128], F32)
        sel = C.sb("sel", [128, NE], F32)
        thr = C.sb("thr", [128, 1], F32)
        cand = C.sb("cand", [128, 1], F32)
        cntp = C.sb("cntp", [128, 1], F32)
        inc = C.sb("inc", [128, 1], F32)
        thr16 = C.sb("thr16", [NE, 1], F32)
        wgt = C.sb("wgt", [NE, OWN], F32)
        S.dma(SP, lambda e: e.dma_start(out=blk[:], in_=blk_in), writes=["blk"])
        S.dma(SP, lambda e: e.dma_start(out=sel[:], in_=sel_in), writes=["sel"])
        S.dma(SP, lambda e: e.dma_start(out=affd, in_=aff[:]), reads=["aff"], writes=["affd"])
        S.dma(SP, lambda e: e.dma_start(out=affF[:], in_=affd.rearrange("e (s t) -> (e s) t", s=8)),
              reads=["affd"], writes=["affF"])
        S.dve(lambda e: e.memset(thr[:], 0.0), writes=["thr"])
        for it in range(1, NITER + 1):
            step = 2.0 ** (-it)
            S.dve(lambda e, step=step: e.tensor_scalar(out=cand[:], in0=thr[:], scalar1=step, scalar2=None,
                                                       op0=ALU.add), reads=["thr"], writes=["cand"])
            S.dve(lambda e: e.tensor_scalar(out=mk[:], in0=affF[:], scalar1=cand[:, 0:1], scalar2=None,
                                            op0=ALU.is_ge), reads=["affF", "cand"], writes=["mk"])
            S.dve(lambda e: e.reduce_sum(out=cntp[:], in_=mk[:], axis=AX.X), reads=["mk"], writes=["cntp"])
            S.pe(lambda e: e.matmul(C.ps[1][:, 0:1], lhsT=blk[:], rhs=cntp[:], start=True, stop=True),
                 reads=["blk", "cntp"], writes=[("ps", 1)])
            S.dve(lambda e, step=step: e.tensor_scalar(out=inc[:], in0=C.ps[1][:, 0:1], scalar1=CAP - 0.5,
                                                       scalar2=step, op0=ALU.is_ge, op1=ALU.mult),
                  reads=[("ps", 1)], writes=["inc"])
            S.dve(lambda e: e.tensor_tensor(out=thr[:], in0=thr[:], in1=inc[:], op=ALU.add),
                  reads=["thr", "inc"], writes=["thr"])
        S.pe(lambda e: e.matmul(C.ps[1][0:NE, 0:1], lhsT=sel[:], rhs=thr[:], start=True, stop=True),
             reads=["sel", "thr"], writes=[("ps", 1)])
        S.dve(lambda e: e.tensor_copy(out=thr16[:], in_=C.ps[1][0:NE, 0:1]), reads=[("ps", 1)], writes=["thr16"])
        S.dve(lambda e: e.scalar_tensor_tensor(out=wgt[:], in0=aff[:, 0:OWN], scalar=thr16[:, 0:1], in1=aff[:, 0:OWN],
                                               op0=ALU.is_ge, op1=ALU.mult), reads=["aff", "thr16"], writes=["wgt"])
        S.dma(SP, lambda e: e.dma_start(out=out, in_=wgt[:]), reads=["wgt"], writes=["OUT"])
        S.emit()
    return nc


def build_experts(final):
    nc = bass.Bass("TRN2", target_bir_lowering=False)
    with ExitStack() as es:
        C = Ctx(nc, es)
        S = C.S
        xT = C.dram_in("xT", [D, OWN], F32)
        wgt_in = C.dram_in("wgt", [NE, OWN], F32)
        mod_in = C.dram_in("modin", [128, 24], F32)
        g_in = C.dram_in("g", [D], F32)
        wg_in = C.dram_in("wg", [NE, D, D], F32)
        wu_in = C.dram_in("wu", [NE, D, D], F32)
        wd_in = C.dram_in("wd", [NE, D, D], F32)
        selE_in = C.dram_in("selE", [NE, NE, 128], F32)
        if final:
            fg_in = C.dram_in("fg", [D], F32)
            fgT = C.sb("fgT", [128, 8], F32)
            S.dma(SP, lambda e: e.dma_start(out=fgT[:], in_=fg_in.rearrange("(k p) -> p k", p=128), allow_slow_non_contiguous=True), writes=["fgT"])
        out = C.dram_out("out", [D, OWN], F32)

        emit_consts(C)
        emit_adaln(C, mod_in, g_in)

        T = 512
        SBK = 1024
        NSB = OWN // SBK
        TB = SBK // T
        PW = 256
        NP = D // PW
        xs = C.sb("xs", [128, 8, T], F32)
        nb = {"sq": C.sb("sq", [128, 8, T], BF16), "rstd": C.sb("rstd", [128, T], F32),
              "tmp": C.sb("tmp", [128, 8, T], F32)}
        hT = C.sb("hT", [128, 8, SBK], BF16)
        wgt = C.sb("wgt", [NE, OWN], BF16)
        selE = C.sb("selE", [NE, NE, 128], BF16)
        acc = C.sb("acc", [128, 8, SBK], F32)
        NR = 3
        wgp = [C.sb("wgp%d" % i, [128, 8, PW], BF16) for i in range(NR)]
        wup = [C.sb("wup%d" % i, [128, 8, PW], BF16) for i in range(NR)]
        wdp = [C.sb("wdp%d" % i, [128, 8, PW], BF16) for i in range(NR)]
        wb = C.sb("wb", [128, SBK], BF16)
        sg = [C.sb("sg%d" % i, [128, T], F32) for i in range(2)]
        hid = C.sb("hid", [128, 8, SBK], BF16)
        S.dma(POOL, lambda e: e.dma_start(out=wgt[:], in_=wgt_in), writes=["wgt"])
        S.dma(POOL, lambda e: e.dma_start(out=selE[:], in_=selE_in), writes=["selE"])
        xv = xT.rearrange("(k p) t -> p k t", p=128)
        ov = out.rearrange("(k p) t -> p k t", p=128)
        pc = 0
        for sbi in range(NSB):
            for tb in range(TB):
                t0 = sbi * SBK + tb * T
                S.dma(SP, lambda e, t0=t0: e.dma_start(out=xs[:], in_=xv[:, :, t0:t0 + T]), writes=["xs"])
                emit_norm(C, xs, "xs", hT[:, :, tb * T:(tb + 1) * T], [("hT", tb, k) for k in range(8)], T, 0, nb)
            for ex in range(NE):
                for tb in range(TB):
                    t0 = sbi * SBK + tb * T
                    S.pe(lambda e, ex=ex, t0=t0: e.matmul(C.ps[0][:], lhsT=selE[:, ex, :], rhs=wgt[:, t0:t0 + T],
                                                          start=True, stop=True),
                         reads=["selE", "wgt"], writes=[("ps", 0)])
                    S.act(lambda e, tb=tb: e.activation(out=wb[:, tb * T:(tb + 1) * T], in_=C.ps[0][:], func=AF.Copy),
                          reads=[("ps", 0)], writes=[("wb", tb)])
                for pi in range(NP):
                    sl = pc % NR
                    pc += 1
                    S.dma(POOL, lambda e, ex=ex, sl=sl, pi=pi: e.dma_start(
                        out=wgp[sl][:], in_=wg_in[ex][:, pi * PW:(pi + 1) * PW].rearrange("(k p) f -> p k f", p=128)),
                        writes=[("wgp", sl)])
                    S.dma(POOL, lambda e, ex=ex, sl=sl, pi=pi: e.dma_start(
                        out=wup[sl][:], in_=wu_in[ex][:, pi * PW:(pi + 1) * PW].rearrange("(k p) f -> p k f", p=128)),
                        writes=[("wup", sl)])
                    for fi in range(PW // 128):
                        f = pi * (PW // 128) + fi
                        for tb in range(TB):
                            par = (f * TB + tb) % 2
                            pg = 2 + par
                            pu = 4 + par

                            def mmg(e, fi=fi, pg=pg, sl=sl, tb=tb):
                                for k in range(8):
                                    r = e.matmul(C.ps[pg][:], lhsT=wgp[sl][:, k, fi * 128:(fi + 1) * 128],
                                                 rhs=hT[:, k, tb * T:(tb + 1) * T], start=(k == 0), stop=(k == 7))
                                return r

                            def mmu(e, fi=fi, pu=pu, sl=sl, tb=tb):
                                for k in range(8):
                                    r = e.matmul(C.ps[pu][:], lhsT=wup[sl][:, k, fi * 128:(fi + 1) * 128],
                                                 rhs=hT[:, k, tb * T:(tb + 1) * T], start=(k == 0), stop=(k == 7))
                                return r
                            hk = [("hT", tb, k) for k in range(8)]
                            S.pe(mmg, reads=[("wgp", sl)] + hk, writes=[("ps", pg)])
                            S.pe(mmu, reads=[("wup", sl)] + hk, writes=[("ps", pu)])
                            S.act(lambda e, pg=pg, par=par: e.activation(out=sg[par][:], in_=C.ps[pg][:], func=AF.Silu),
                                  reads=[("ps", pg)], writes=[("sg", par)])
                            S.dve(lambda e, pu=pu, par=par: e.tensor_tensor(out=sg[par][:], in0=C.ps[pu][:],
                                                                             in1=sg[par][:], op=ALU.mult),
                                  reads=[("ps", pu), ("sg", par)], writes=[("sg", par)])
                            S.pool(lambda e, f=f, tb=tb, par=par: e.tensor_tensor(
                                out=hid[:, f, tb * T:(tb + 1) * T], in0=sg[par][:], in1=wb[:, tb * T:(tb + 1) * T],
                                op=ALU.mult), reads=[("sg", par), ("wb", tb)], writes=[("hid", f, tb)])
                for pi in range(NP):
                    sl = pc % NR
                    pc += 1
                    S.dma(POOL, lambda e, ex=ex, sl=sl, pi=pi: e.dma_start(
                        out=wdp[sl][:], in_=wd_in[ex][:, pi * PW:(pi + 1) * PW].rearrange("(k p) f -> p k f", p=128)),
                        writes=[("wdp", sl)])
                    for di in range(PW // 128):
                        dch = pi * (PW // 128) + di
                        for tb in range(TB):
                            py = 6 + ((dch * TB + tb) % 2)

                            def mmd(e, di=di, py=py, sl=sl, tb=tb):
                                for f in range(8):
                                    r = e.matmul(C.ps[py][:], lhsT=wdp[sl][:, f, di * 128:(di + 1) * 128],
                                                 rhs=hid[:, f, tb * T:(tb + 1) * T], start=(f == 0), stop=(f == 7))
                                return r
                            S.pe(mmd, reads=[("wdp", sl)] + [("hid", f, tb) for f in range(8)], writes=[("ps", py)])
                            a_sl = acc[:, dch, tb * T:(tb + 1) * T]
                            if ex == 0:
                                S.dve(lambda e, py=py, a_sl=a_sl: e.tensor_copy(out=a_sl, in_=C.ps[py][:]),
                                      reads=[("ps", py)], writes=[("acc", dch, tb)])
                            else:
                                S.dve(lambda e, py=py, a_sl=a_sl: e.tensor_tensor(out=a_sl, in0=a_sl, in1=C.ps[py][:],
                                                                                   op=ALU.add),
                                      reads=[("ps", py), ("acc", dch, tb)], writes=[("acc", dch, tb)])
            for tb in range(TB):
                t0 = sbi * SBK + tb * T
                S.dma(SP, lambda e, t0=t0: e.dma_start(out=xs[:], in_=xv[:, :, t0:t0 + T]), writes=["xs"])
                for dch in range(8):
                    S.dve(lambda e, dch=dch, tb=tb: e.scalar_tensor_tensor(
                        out=acc[:, dch, tb * T:(tb + 1) * T], in0=acc[:, dch, tb * T:(tb + 1) * T],
                        scalar=C.mod[:, 16 + dch:17 + dch], in1=xs[:, dch, :], op0=ALU.mult, op1=ALU.add),
                        reads=[("acc", dch, tb), "xs", "mod"], writes=[("acc", dch, tb)])
                if not final:
                    S.dma(SP, lambda e, t0=t0, tb=tb: e.dma_start(out=ov[:, :, t0:t0 + T],
                                                                  in_=acc[:, :, tb * T:(tb + 1) * T]),
                          reads=[("acc", dch, tb) for dch in range(8)], writes=[("OUT", tb)], sem_key=("accst", tb))
                else:
                    fo = nb["tmp"]
                    emit_norm(C, acc[:, :, tb * T:(tb + 1) * T], ("acc", 0, tb), fo, [("fo", k) for k in range(8)],
                              T, 1, nb, amul=fgT, akey="fgT", shift=None,
                              xkeys=[("acc", dch, tb) for dch in range(8)])
                    S.dma(SP, lambda e, t0=t0: e.dma_start(out=ov[:, :, t0:t0 + T], in_=fo[:]),
                          reads=[("fo", k) for k in range(8)], writes=["OUTF"], sem_key="fost")
        S.emit()
    return nc


def build_moe4(final):
    nc = bass.Bass("TRN2", target_bir_lowering=False)
    with ExitStack() as es:
        C = Ctx(nc, es)
        S = C.S
        xT = C.dram_in("xT", [D, SEQ], F32)
        mod_in = C.dram_in("modin", [128, 24], F32)
        g_in = C.dram_in("g", [D], F32)
        rw_in = C.dram_in("rw", [D, NE], F32)
        rb_in = C.dram_in("rb", [NE], F32)
        blk_in = C.dram_in("blkones", [128, 128], F32)
        sel_in = C.dram_in("sel", [128, NE], F32)
        affd = nc.dram_tensor("affd", [NE, SEQ], F32, kind="Internal").ap()

        wg_in = C.dram_in("wg", [NE, D, D], F32)
        wu_in = C.dram_in("wu", [NE, D, D], F32)
        wd_in = C.dram_in("wd", [NE, D, D], F32)
        selE_in = C.dram_in("selE", [NE, NE, 128], F32)
        if final:
            fg_in = C.dram_in("fg", [D], F32)
            fgT = C.sb("fgT", [128, 8], F32)
            S.dma(SP, lambda e: e.dma_start(out=fgT[:], in_=fg_in.rearrange("(k p) -> p k", p=128),
                                            allow_slow_non_contiguous=True), writes=["fgT"])
        out = C.dram_out("out", [D, SEQ], F32)
        emit_consts(C)
        emit_adaln(C, mod_in, g_in)
        wgt = C.sb("wgt", [NE, SEQ], BF16)
        selE = C.sb("selE", [NE, NE, 128], BF16)
        S.dma(POOL, lambda e: e.dma_start(out=selE[:], in_=selE_in), writes=["selE"])
        mk0 = C.mark()

        T = 512
        NB = SEQ // T
        xsA = [C.sb("xsA%d" % i, [128, 8, T], F32) for i in range(2)]
        nbA = {"sq": C.sb("sq", [128, 8, T], BF16), "rstd": C.sb("rstd", [128, T], F32),
              "tmp": C.sb("tmp", [128, 8, T], F32)}
        hf = C.sb("hf", [128, 8, T], F32)
        rw = C.sb("rw", [128, 8, NE], F32)
        rb = C.sb("rb", [NE, 1], F32)
        ones16 = C.sb("ones16", [NE, NE], F32)
        Eexp = C.sb("Eexp", [NE, T], F32)
        rcp = C.sb("rcp", [NE, T], F32)
        aff = C.sb("aff", [NE, SEQ], F32)
        S.dma(SP, lambda e: e.dma_start(out=rw[:], in_=rw_in.rearrange("(k p) n -> p k n", p=128)), writes=["rw"])
        S.dma(SP, lambda e: e.dma_start(out=rb[:], in_=rb_in.rearrange("(p o) -> p o", o=1)), writes=["rb"])
        S.dve(lambda e: e.memset(ones16[:], 1.0), writes=["ones16"])

        xvA = xT.rearrange("(k p) t -> p k t", p=128)
        for j in range(NB):
            sl = j % 2
            S.dma(SP, lambda e, j=j, sl=sl: e.dma_start(out=xsA[sl][:], in_=xvA[:, :, j * T:(j + 1) * T]),
                  writes=[("xsA", sl)])
            emit_norm(C, xsA[sl], ("xsA", sl), hf, [("hf", k) for k in range(8)], T, 0, nbA)

            def mm(e):
                for k in range(8):
                    r = e.matmul(C.ps[1][0:NE, :], lhsT=rw[:, k, :], rhs=hf[:, k, :], start=(k == 0), stop=(k == 7))
                return r
            S.pe(mm, reads=["rw"] + [("hf", k) for k in range(8)], writes=[("ps", 1)])
            S.act(lambda e: e.activation(out=Eexp[:], in_=C.ps[1][0:NE, :], func=AF.Exp, bias=rb[:, 0:1], scale=1.0),
                  reads=[("ps", 1), "rb"], writes=["Eexp"])
            S.pe(lambda e: e.matmul(C.ps[2][0:NE, :], lhsT=ones16[:], rhs=Eexp[:], start=True, stop=True),
                 reads=["ones16", "Eexp"], writes=[("ps", 2)])
            S.dve(lambda e: e.reciprocal(out=rcp[:], in_=C.ps[2][0:NE, :]), reads=[("ps", 2)], writes=["rcp"])
            S.dve(lambda e, j=j: e.tensor_tensor(out=aff[:, j * T:(j + 1) * T], in0=Eexp[:], in1=rcp[:], op=ALU.mult),
                  reads=["Eexp", "rcp"], writes=["aff"])

        affF = C.sb("affF", [128, SEQ // 8], F32)
        mk = C.sb("mk", [128, SEQ // 8], F32)
        blk = C.sb("blk", [128, 128], F32)
        sel = C.sb("sel", [128, NE], F32)
        thr = C.sb("thr", [128, 1], F32)
        cand = C.sb("cand", [128, 1], F32)
        cntp = C.sb("cntp", [128, 1], F32)
        inc = C.sb("inc", [128, 1], F32)
        thr16 = C.sb("thr16", [NE, 1], F32)
        S.dma(SP, lambda e: e.dma_start(out=blk[:], in_=blk_in), writes=["blk"])
        S.dma(SP, lambda e: e.dma_start(out=sel[:], in_=sel_in), writes=["sel"])
        S.dma(SP, lambda e: e.dma_start(out=affd, in_=aff[:]), reads=["aff"], writes=["affd"])
        S.dma(SP, lambda e: e.dma_start(out=affF[:], in_=affd.rearrange("e (s t) -> (e s) t", s=8)),
              reads=["affd"], writes=["affF"])
        S.dve(lambda e: e.memset(thr[:], 0.0), writes=["thr"])
        for it in range(1, NITER + 1):
            step = 2.0 ** (-it)
            S.dve(lambda e, step=step: e.tensor_scalar(out=cand[:], in0=thr[:], scalar1=step, scalar2=None,
                                                       op0=ALU.add), reads=["thr"], writes=["cand"])
            S.dve(lambda e: e.tensor_scalar(out=mk[:], in0=affF[:], scalar1=cand[:, 0:1], scalar2=None,
                                            op0=ALU.is_ge), reads=["affF", "cand"], writes=["mk"])
            S.dve(lambda e: e.reduce_sum(out=cntp[:], in_=mk[:], axis=AX.X), reads=["mk"], writes=["cntp"])
            S.pe(lambda e: e.matmul(C.ps[1][:, 0:1], lhsT=blk[:], rhs=cntp[:], start=True, stop=True),
                 reads=["blk", "cntp"], writes=[("ps", 1)])
            S.dve(lambda e, step=step: e.tensor_scalar(out=inc[:], in0=C.ps[1][:, 0:1], scalar1=CAP - 0.5,
                                                       scalar2=step, op0=ALU.is_ge, op1=ALU.mult),
                  reads=[("ps", 1)], writes=["inc"])
            S.dve(lambda e: e.tensor_tensor(out=thr[:], in0=thr[:], in1=inc[:], op=ALU.add),
                  reads=["thr", "inc"], writes=["thr"])
        S.pe(lambda e: e.matmul(C.ps[1][0:NE, 0:1], lhsT=sel[:], rhs=thr[:], start=True, stop=True),
             reads=["sel", "thr"], writes=[("ps", 1)])
        S.dve(lambda e: e.tensor_copy(out=thr16[:], in_=C.ps[1][0:NE, 0:1]), reads=[("ps", 1)], writes=["thr16"])
        S.dve(lambda e: e.scalar_tensor_tensor(out=wgt[:], in0=aff[:], scalar=thr16[:, 0:1], in1=aff[:],
                                               op0=ALU.is_ge, op1=ALU.mult), reads=["aff", "thr16"], writes=["wgt"])
        S.barrier()
        C.reset(mk0)
        T = 512
        SBK = 1024
        NSB = SEQ // SBK
        TB = SBK // T
        PW = 256
        NP = D // PW
        xs = C.sb("xs", [128, 8, T], F32)
        nb = {"sq": C.sb("sq", [128, 8, T], BF16), "rstd": C.sb("rstd", [128, T], F32),
              "tmp": C.sb("tmp", [128, 8, T], F32)}
        hT = C.sb("hT", [128, 8, SBK], BF16)
        acc = C.sb("acc", [128, 8, SBK], F32)
        NR = 3
        wgp = [C.sb("wgp%d" % i, [128, 8, PW], BF16) for i in range(NR)]
        wup = [C.sb("wup%d" % i, [128, 8, PW], BF16) for i in range(NR)]
        wdp = [C.sb("wdp%d" % i, [128, 8, PW], BF16) for i in range(NR)]
        wb = C.sb("wb", [128, SBK], BF16)
        sg = [C.sb("sg%d" % i, [128, T], F32) for i in range(2)]
        hid = C.sb("hid", [128, 8, SBK], BF16)
        xv = xT.rearrange("(k p) t -> p k t", p=128)
        ov = out.rearrange("(k p) t -> p k t", p=128)
        pc = 0
        for sbi in range(NSB):
            for tb in range(TB):
                t0 = sbi * SBK + tb * T
                S.dma(SP, lambda e, t0=t0: e.dma_start(out=xs[:], in_=xv[:, :, t0:t0 + T]), writes=["xs"])
                emit_norm(C, xs, "xs", hT[:, :, tb * T:(tb + 1) * T], [("hT", tb, k) for k in range(8)], T, 0, nb)
            for ex in range(NE):
                for tb in range(TB):
                    t0 = sbi * SBK + tb * T
                    S.pe(lambda e, ex=ex, t0=t0: e.matmul(C.ps[0][:], lhsT=selE[:, ex, :], rhs=wgt[:, t0:t0 + T],
                                                          start=True, stop=True),
                         reads=["selE", "wgt"], writes=[("ps", 0)])
                    S.act(lambda e, tb=tb: e.activation(out=wb[:, tb * T:(tb + 1) * T], in_=C.ps[0][:], func=AF.Copy),
                          reads=[("ps", 0)], writes=[("wb", tb)])
                for pi in range(NP):
                    sl = pc % NR
                    pc += 1
                    S.dma(POOL, lambda e, ex=ex, sl=sl, pi=pi: e.dma_start(
                        out=wgp[sl][:], in_=wg_in[ex][:, pi * PW:(pi + 1) * PW].rearrange("(k p) f -> p k f", p=128)),
                        writes=[("wgp", sl)])
                    S.dma(POOL, lambda e, ex=ex, sl=sl, pi=pi: e.dma_start(
                        out=wup[sl][:], in_=wu_in[ex][:, pi * PW:(pi + 1) * PW].rearrange("(k p) f -> p k f", p=128)),
                        writes=[("wup", sl)])
                    for fi in range(PW // 128):
                        f = pi * (PW // 128) + fi
                        for tb in range(TB):
                            par = (f * TB + tb) % 2
                            pg = 2 + par
                            pu = 4 + par

                            def mmg(e, fi=fi, pg=pg, sl=sl, tb=tb):
                                for k in range(8):
                                    r = e.matmul(C.ps[pg][:], lhsT=wgp[sl][:, k, fi * 128:(fi + 1) * 128],
                                                 rhs=hT[:, k, tb * T:(tb + 1) * T], start=(k == 0), stop=(k == 7))
                                return r

                            def mmu(e, fi=fi, pu=pu, sl=sl, tb=tb):
                                for k in range(8):
                                    r = e.matmul(C.ps[pu][:], lhsT=wup[sl][:, k, fi * 128:(fi + 1) * 128],
                                                 rhs=hT[:, k, tb * T:(tb + 1) * T], start=(k == 0), stop=(k == 7))
                                return r
                            hk = [("hT", tb, k) for k in range(8)]
                            S.pe(mmg, reads=[("wgp", sl)] + hk, writes=[("ps", pg)])
                            S.pe(mmu, reads=[("wup", sl)] + hk, writes=[("ps", pu)])
                            S.act(lambda e, pg=pg, par=par: e.activation(out=sg[par][:], in_=C.ps[pg][:], func=AF.Silu),
                                  reads=[("ps", pg)], writes=[("sg", par)])
                            S.dve(lambda e, pu=pu, par=par: e.tensor_tensor(out=sg[par][:], in0=C.ps[pu][:],
                                                                             in1=sg[par][:], op=ALU.mult),
                                  reads=[("ps", pu), ("sg", par)], writes=[("sg", par)])
                            S.pool(lambda e, f=f, tb=tb, par=par: e.tensor_tensor(
                                out=hid[:, f, tb * T:(tb + 1) * T], in0=sg[par][:], in1=wb[:, tb * T:(tb + 1) * T],
                                op=ALU.mult), reads=[("sg", par), ("wb", tb)], writes=[("hid", f, tb)])
                for pi in range(NP):
                    sl = pc % NR
                    pc += 1
                    S.dma(POOL, lambda e, ex=ex, sl=sl, pi=pi: e.dma_start(
                        out=wdp[sl][:], in_=wd_in[ex][:, pi * PW:(pi + 1) * PW].rearrange("(k p) f -> p k f", p=128)),
                        writes=[("wdp", sl)])
                    for di in range(PW // 128):
                        dch = pi * (PW // 128) + di
                        for tb in range(TB):
                            py = 6 + ((dch * TB + tb) % 2)

                            def mmd(e, di=di, py=py, sl=sl, tb=tb):
                                for f in range(8):
                                    r = e.matmul(C.ps[py][:], lhsT=wdp[sl][:, f, di * 128:(di + 1) * 128],
                                                 rhs=hid[:, f, tb * T:(tb + 1) * T], start=(f == 0), stop=(f == 7))
                                return r
                            S.pe(mmd, reads=[("wdp", sl)] + [("hid", f, tb) for f in range(8)], writes=[("ps", py)])
                            a_sl = acc[:, dch, tb * T:(tb + 1) * T]
                            if ex == 0:
                                S.dve(lambda e, py=py, a_sl=a_sl: e.tensor_copy(out=a_sl, in_=C.ps[py][:]),
                                      reads=[("ps", py)], writes=[("acc", dch, tb)])
                            else:
                                S.dve(lambda e, py=py, a_sl=a_sl: e.tensor_tensor(out=a_sl, in0=a_sl, in1=C.ps[py][:],
                                                                                   op=ALU.add),
                                      reads=[("ps", py), ("acc", dch, tb)], writes=[("acc", dch, tb)])
            for tb in range(TB):
                t0 = sbi * SBK + tb * T
                S.dma(SP, lambda e, t0=t0: e.dma_start(out=xs[:], in_=xv[:, :, t0:t0 + T]), writes=["xs"])
                for dch in range(8):
                    S.dve(lambda e, dch=dch, tb=tb: e.scalar_tensor_tensor(
                        out=acc[:, dch, tb * T:(tb + 1) * T], in0=acc[:, dch, tb * T:(tb + 1) * T],
                        scalar=C.mod[:, 16 + dch:17 + dch], in1=xs[:, dch, :], op0=ALU.mult, op1=ALU.add),
                        reads=[("acc", dch, tb), "xs", "mod"], writes=[("acc", dch, tb)])
                if not final:
                    S.dma(SP, lambda e, t0=t0, tb=tb: e.dma_start(out=ov[:, :, t0:t0 + T],
                                                                  in_=acc[:, :, tb * T:(tb + 1) * T]),
                          reads=[("acc", dch, tb) for dch in range(8)], writes=[("OUT", tb)], sem_key=("accst", tb))
                else:
                    fo = nb["tmp"]
                    emit_norm(C, acc[:, :, tb * T:(tb + 1) * T], ("acc", 0, tb), fo, [("fo", k) for k in range(8)],
                              T, 1, nb, amul=fgT, akey="fgT", shift=None,
                              xkeys=[("acc", dch, tb) for dch in range(8)])
                    S.dma(SP, lambda e, t0=t0: e.dma_start(out=ov[:, :, t0:t0 + T], in_=fo[:]),
                          reads=[("fo", k) for k in range(8)], writes=["OUTF"], sem_key="fost")
        S.emit()
    return nc


KB = 256
HALO = 15


def build_even():
    nc = bass.Bass("TRN2", target_bir_lowering=False)
    with ExitStack() as es:
        C = Ctx(nc, es)
        S = C.S
        xT = C.dram_in("xT", [D, SEQ], F32)
        mod_in = C.dram_in("modin", [128, 24], F32)
        g_in = C.dram_in("g", [D], F32)
        win_in = C.dram_in("w_in", [D, 1536], F32)
        wout_in = C.dram_in("w_out", [D, D], F32)
        cw_in = C.dram_in("conv_w", [31, 512], F32)
        cb_in = C.dram_in("conv_b", [512], F32)
        lg_in = C.dram_in("ln_g", [512], F32)
        lb_in = C.dram_in("ln_b", [512], F32)
        dC_in = C.dram_in("dftC", [SEQ, OWN], BF16)
        dS_in = C.dram_in("dftNS", [SEQ, OWN], BF16)
        cs_in = C.dram_in("cs128", [128, 256], BF16)
        fl_in = C.dram_in("flags", [128, 2], F32)
        id_in = C.dram_in("ident", [128, 128], F32)
        out = C.dram_out("out", [D, OWN], F32)

        emit_consts(C)
        emit_adaln(C, mod_in, g_in)
        uf = C.sb("uf", [128, SEQ // 128, 512], BF16)
        GLW = OWN + 2 * HALO
        gl = C.sb("gl", [128, 4, GLW], BF16)
        wout = C.sb("wout", [128, 8, D], BF16)
        cs128 = C.sb("cs128", [128, 256], BF16)
        flags = C.sb("flags", [128, 2], F32)
        ident = C.sb("ident", [128, 128], F32)
        cwT = C.sb("cwT", [128, 4, 31], F32)
        cbT = C.sb("cbT", [128, 4], F32)
        lgT = C.sb("lgT", [128, 4], F32)
        lbT = C.sb("lbT", [128, 4], F32)
        ones512 = C.sb("ones512", [128, 128], BF16)
        S.dma(POOL, lambda e: e.dma_start(out=wout[:], in_=wout_in.rearrange("(k p) n -> p k n", p=128)), writes=["wout"])
        S.dma(SP, lambda e: e.dma_start(out=cs128[:], in_=cs_in), writes=["cs128"])
        S.dma(SP, lambda e: e.dma_start(out=flags[:], in_=fl_in), writes=["flags"])
        S.dma(SP, lambda e: e.dma_start(out=ident[:], in_=id_in), writes=["ident"])
        for cc in range(4):
            S.dma(SP, lambda e, cc=cc: e.dma_start(
                out=cwT[:, cc, :], in_=cw_in[:, cc * 128:(cc + 1) * 128].rearrange("j p -> p j"),
                allow_slow_non_contiguous=True), writes=[("cwT", cc)])
        for (t, src, nm) in ((cbT, cb_in, "cbT"), (lgT, lg_in, "lgT"), (lbT, lb_in, "lbT")):
            S.dma(SP, lambda e, t=t, src=src: e.dma_start(out=t[:], in_=src.rearrange("(k p) -> p k", p=128),
                                                         allow_slow_non_contiguous=True), writes=[nm])
        S.dve(lambda e: e.memset(ones512[:], 1.0 / 512), writes=["ones512"])
        mk0 = C.mark()
        T = 512
        xs = C.sb("xs", [128, 8, T], F32)
        nb = {"sq": C.sb("sq", [128, 8, T], BF16), "rstd": C.sb("rstd", [128, T], F32),
              "tmp": C.sb("tmp", [128, 8, T], F32)}
        hT = C.sb("hT", [128, 8, T], BF16)
        win = C.sb("win", [128, 8, 1536], BF16)
        sig = [C.sb("sig%d" % i, [128, T], F32) for i in range(2)]
        S.dma(POOL, lambda e: e.dma_start(out=win[:], in_=win_in.rearrange("(k p) n -> p k n", p=128)), writes=["win"])
        xv = xT.rearrange("(k p) t -> p k t", p=128)
        hk = [("hT", k) for k in range(8)]
        for j in range(SEQ // T):
            S.dma(SP, lambda e, j=j: e.dma_start(out=xs[:], in_=xv[:, :, j * T:(j + 1) * T]), writes=["xs"])
            emit_norm(C, xs, "xs", hT, hk, T, 0, nb)
            for tt in range(4):
                pb = 1 + (tt % 2)

                def mmf(e, tt=tt, pb=pb):
                    for k in range(8):
                        r = e.matmul(C.ps[pb][:], lhsT=hT[:, k, tt * 128:(tt + 1) * 128], rhs=win[:, k, 0:512],
                                     start=(k == 0), stop=(k == 7))
                    return r
                S.pe(mmf, reads=["win"] + hk, writes=[("ps", pb)])
                S.act(lambda e, j=j, tt=tt, pb=pb: e.activation(out=uf[:, j * 4 + tt, :], in_=C.ps[pb][:], func=AF.Copy),
                      reads=[("ps", pb)], writes=[("uf", j * 4 + tt)])
            if j < OWN // T or j in (OWN // T, SEQ // T - 1):
                for cc in range(4):
                    pv = 3 + (cc % 2)
                    pg = 5 + (cc % 2)
                    s2 = cc % 2

                    def mmv(e, cc=cc, pv=pv):
                        for k in range(8):
                            r = e.matmul(C.ps[pv][:], lhsT=win[:, k, 512 + cc * 128:512 + (cc + 1) * 128], rhs=hT[:, k, :],
                                         start=(k == 0), stop=(k == 7))
                        return r

                    def mmg(e, cc=cc, pg=pg):
                        for k in range(8):
                            r = e.matmul(C.ps[pg][:], lhsT=win[:, k, 1024 + cc * 128:1024 + (cc + 1) * 128], rhs=hT[:, k, :],
                                         start=(k == 0), stop=(k == 7))
                        return r
                    S.pe(mmv, reads=["win"] + hk, writes=[("ps", pv)])
                    S.pe(mmg, reads=["win"] + hk, writes=[("ps", pg)])
                    S.act(lambda e, pg=pg, s2=s2: e.activation(out=sig[s2][:], in_=C.ps[pg][:], func=AF.Sigmoid),
                          reads=[("ps", pg)], writes=[("sig", s2)])
                    if j < OWN // T:
                        S.dve(lambda e, cc=cc, pv=pv, s2=s2, j=j: e.tensor_tensor(
                            out=gl[:, cc, HALO + j * T:HALO + (j + 1) * T], in0=C.ps[pv][:], in1=sig[s2][:], op=ALU.mult),
                            reads=[("ps", pv), ("sig", s2)], writes=[("gl", cc, j)])
                    else:
                        S.dve(lambda e, pv=pv, s2=s2: e.tensor_tensor(out=sig[s2][:], in0=C.ps[pv][:], in1=sig[s2][:],
                                                                      op=ALU.mult),
                              reads=[("ps", pv), ("sig", s2)], writes=[("sig", s2)])
                        if j == OWN // T:
                            S.dve(lambda e, cc=cc, s2=s2: e.tensor_scalar(
                                out=gl[:, cc, HALO + OWN:HALO + OWN + HALO], in0=sig[s2][:, 0:HALO],
                                scalar1=flags[:, 1:2], scalar2=None, op0=ALU.mult),
                                reads=[("sig", s2), "flags"], writes=[("gl", cc, "R")])
                        else:
                            S.dve(lambda e, cc=cc, s2=s2: e.tensor_scalar(
                                out=gl[:, cc, 0:HALO], in0=sig[s2][:, T - HALO:T],
                                scalar1=flags[:, 0:1], scalar2=None, op0=ALU.mult),
                                reads=[("sig", s2), "flags"], writes=[("gl", cc, "L")])
        S.barrier()
        C.reset(mk0)
        NSG = 8
        dCp = [C.sb("dCp%d" % i, [128, NSG, KB], BF16) for i in range(2)]
        dSp = [C.sb("dSp%d" % i, [128, NSG, KB], BF16) for i in range(2)]
        diag = C.sb("diag", [128, 4, 31, 128], BF16)
        pq = C.sb("pq", [128, 4, 2 * KB], BF16)
        yf = C.sb("yf", [128, 4, KB], BF16)
        conv = C.sb("conv", [128, 4, KB], F32)
        convb = C.sb("convb", [128, 4, KB], BF16)
        dd = C.sb("dd", [128, 4, KB], F32)
        sq2 = C.sb("sq2", [128, 4, KB], BF16)
        rs2 = C.sb("rs2", [128, KB], F32)
        yc = C.sb("yc", [128, 4, KB], BF16)
        xs2 = C.sb("xs2", [128, 8, KB], F32)
        xo = C.sb("xo", [128, 8, KB], F32)
        for cc in range(4):
            for j in range(31):
                S.dve(lambda e, cc=cc, j=j: e.tensor_scalar(out=diag[:, cc, j, :], in0=ident[:],
                                                            scalar1=cwT[:, cc, j:j + 1], scalar2=None, op0=ALU.mult),
                      reads=["ident", ("cwT", cc)], writes=[("diag", cc)])
        dCv = dC_in.rearrange("(c p) k -> p c k", p=128)
        dSv = dS_in.rearrange("(c p) k -> p c k", p=128)
        ov = out.rearrange("(k p) t -> p k t", p=128)
        pcnt = 0
        for kb in range(OWN // KB):
            k0 = kb * KB
            for sg in range(SEQ // 128 // NSG):
                sl = pcnt % 2
                pcnt += 1
                S.dma(SP, lambda e, sg=sg, sl=sl, k0=k0: e.dma_start(
                    out=dCp[sl][:], in_=dCv[:, sg * NSG:(sg + 1) * NSG, k0:k0 + KB]), writes=[("dCp", sl)])
                S.dma(SP, lambda e, sg=sg, sl=sl, k0=k0: e.dma_start(
                    out=dSp[sl][:], in_=dSv[:, sg * NSG:(sg + 1) * NSG, k0:k0 + KB]), writes=[("dSp", sl)])

                def mmd(e, sg=sg, sl=sl):
                    for sc in range(NSG):
                        ti = sg * NSG + sc
                        first = (ti == 0)
                        last = (ti == SEQ // 128 - 1)
                        for g in range(4):
                            e.matmul(C.ps[g][:, 0:KB], lhsT=uf[:, ti, g * 128:(g + 1) * 128], rhs=dCp[sl][:, sc, :],
                                     start=first, stop=last, skip_group_check=True)
                            r = e.matmul(C.ps[g][:, KB:2 * KB], lhsT=uf[:, ti, g * 128:(g + 1) * 128],
                                         rhs=dSp[sl][:, sc, :], start=False, stop=last, skip_group_check=True)
                    return r
                S.pe(mmd, reads=[("dCp", sl), ("dSp", sl)] + [("uf", sg * NSG + sc) for sc in range(NSG)],
                     writes=[("ps", g) for g in range(4)])
            for g in range(4):
                S.act(lambda e, g=g: e.activation(out=pq[:, g, :], in_=C.ps[g][:], func=AF.Copy),
                      reads=[("ps", g)], writes=[("pq", g)])
            for pb in (4, 5):
                def mmc(e, pb=pb):
                    for gg in range(2):
                        g = (pb - 4) * 2 + gg
                        c0 = gg * KB
                        e.matmul(C.ps[pb][:, c0:c0 + KB], lhsT=cs128[:, 0:128], rhs=pq[:, g, 0:KB], start=True, stop=False,
                                 skip_group_check=True)
                        r = e.matmul(C.ps[pb][:, c0:c0 + KB], lhsT=cs128[:, 128:256], rhs=pq[:, g, KB:2 * KB],
                                     start=False, stop=True, skip_group_check=True)
                    return r
                S.pe(mmc, reads=["cs128", ("pq", (pb - 4) * 2), ("pq", (pb - 4) * 2 + 1)], writes=[("ps", pb)])
                for gg in range(2):
                    g = (pb - 4) * 2 + gg
                    c0 = gg * KB
                    S.act(lambda e, g=g, pb=pb, c0=c0: e.activation(out=yf[:, g, :], in_=C.ps[pb][:, c0:c0 + KB],
                                                                     func=AF.Copy, scale=1.0 / 1024),
                          reads=[("ps", pb)], writes=[("yf", g)])
            for cc in range(4):
                c0 = 0
                pcv = 6 + (cc % 2)

                def mmv(e, cc=cc, pcv=pcv, k0=k0):
                    for j in range(31):
                        r = e.matmul(C.ps[pcv][:, 0:KB], lhsT=diag[:, cc, j, :], rhs=gl[:, cc, k0 + j:k0 + j + KB],
                                     start=(j == 0), stop=(j == 30))
                    return r
                glk = [("gl", cc, jj) for jj in range(OWN // 512)] + [("gl", cc, "L"), ("gl", cc, "R")]
                S.pe(mmv, reads=[("diag", cc)] + glk, writes=[("ps", pcv)])
                S.act(lambda e, cc=cc, pcv=pcv: e.activation(out=conv[:, cc, :], in_=C.ps[pcv][:, 0:KB],
                                                             func=AF.Identity, bias=cbT[:, cc:cc + 1], scale=1.0),
                      reads=[("ps", pcv), "cbT"], writes=[("conv", cc)])
                S.pool(lambda e, cc=cc: e.tensor_copy(out=convb[:, cc, :], in_=conv[:, cc, :]),
                       reads=[("conv", cc)], writes=[("convb", cc)])

            def mmm(e):
                for cc in range(4):
                    r = e.matmul(C.ps[6][:, 0:KB], lhsT=ones512[:], rhs=convb[:, cc, :], start=(cc == 0), stop=(cc == 3))
                return r
            S.pe(mmm, reads=["ones512"] + [("convb", cc) for cc in range(4)], writes=[("ps", 6)])
            for cc in range(4):
                S.dve(lambda e, cc=cc: e.tensor_tensor(out=dd[:, cc, :], in0=conv[:, cc, :], in1=C.ps[6][:, 0:KB],
                                                       op=ALU.subtract),
                      reads=[("conv", cc), ("ps", 6)], writes=[("dd", cc)])
                S.act(lambda e, cc=cc: e.activation(out=sq2[:, cc, :], in_=dd[:, cc, :], func=AF.Square),
                      reads=[("dd", cc)], writes=[("sq2", cc)])

            def mmvv(e):
                for cc in range(4):
                    r = e.matmul(C.ps[7][:, 0:KB], lhsT=ones512[:], rhs=sq2[:, cc, :], start=(cc == 0), stop=(cc == 3))
                return r
            S.pe(mmvv, reads=["ones512"] + [("sq2", cc) for cc in range(4)], writes=[("ps", 7)])
            S.act(lambda e: e.activation(out=rs2[:], in_=C.ps[7][:, 0:KB], func=AF.Sqrt, bias=C.epst[:, 0:1], scale=1.0),
                  reads=[("ps", 7), "epst"], writes=["rs2"])
            S.dve(lambda e: e.reciprocal(out=rs2[:], in_=rs2[:]), reads=["rs2"], writes=["rs2"])
            for cc in range(4):
                S.dve(lambda e, cc=cc: e.tensor_tensor(out=dd[:, cc, :], in0=dd[:, cc, :], in1=rs2[:], op=ALU.mult),
                      reads=[("dd", cc), "rs2"], writes=[("dd", cc)])
                S.act(lambda e, cc=cc: e.activation(out=yc[:, cc, :], in_=dd[:, cc, :], func=AF.Silu,
                                                    bias=lbT[:, cc:cc + 1], scale=lgT[:, cc:cc + 1]),
                      reads=[("dd", cc), "lbT", "lgT"], writes=[("yc", cc)])
            S.dma(SP, lambda e, k0=k0: e.dma_start(out=xs2[:], in_=xv[:, :, k0:k0 + KB]), writes=["xs2"])
            for dch in range(8):
                pb = 4 + (dch % 2)
                c0 = 0

                def mmo(e, dch=dch, pb=pb):
                    for kc in range(8):
                        rhs = yf[:, kc, :] if kc < 4 else yc[:, kc - 4, :]
                        r = e.matmul(C.ps[pb][:, 0:KB], lhsT=wout[:, kc, dch * 128:(dch + 1) * 128], rhs=rhs,
                                     start=(kc == 0), stop=(kc == 7))
                    return r
                S.pe(mmo, reads=["wout"] + [("yf", g) for g in range(4)] + [("yc", cc) for cc in range(4)],
                     writes=[("ps", pb)])
                S.dve(lambda e, dch=dch, pb=pb: e.scalar_tensor_tensor(
                    out=xo[:, dch, :], in0=C.ps[pb][:, 0:KB], scalar=C.mod[:, 16 + dch:17 + dch], in1=xs2[:, dch, :],
                    op0=ALU.mult, op1=ALU.add), reads=[("ps", pb), "xs2", "mod"], writes=[("xo", dch)])
            S.dma(SP, lambda e, k0=k0: e.dma_start(out=ov[:, :, k0:k0 + KB], in_=xo[:]),
                  reads=[("xo", dch) for dch in range(8)], writes=["OUT"], sem_key="xost")
        S.emit()
    return nc


NKT = OWN // 128 + 2
TWO_PI = float(2.0 * np.pi)


def emit_rope_tables(C, posrep, t0, T, bufs, tag):
    S = C.S
    pi_, pf, r1, r2, CT, ST = bufs["pi"], bufs["pf"], bufs["r1"], bufs["r2"], bufs["CT"], bufs["ST"]
    C1 = 6.28125
    C2 = float(2.0 * np.pi - 6.28125)
    PI = float(np.pi)
    S.dma(SP, lambda e: e.dma_start(out=pi_[:, :T], in_=posrep[:, t0:t0 + T]), writes=["pi" + tag])
    S.dve(lambda e: e.tensor_copy(out=pf[:, :T], in_=pi_[:, :T]), reads=["pi" + tag], writes=["pf" + tag])
    S.dve(lambda e: e.tensor_scalar(out=pf[:, :T], in0=pf[:, :T], scalar1=C.invf[:, 0:1], scalar2=None, op0=ALU.mult),
          reads=["pf" + tag, "invf"], writes=["pf" + tag])
    S.dve(lambda e: e.tensor_scalar(out=pi_[:, :T], in0=pf[:, :T], scalar1=float(1.0 / (2.0 * np.pi)), scalar2=None,
                                    op0=ALU.mult), reads=["pf" + tag], writes=["pi" + tag])
    S.dve(lambda e: e.tensor_copy(out=r2[:, :T], in_=pi_[:, :T]), reads=["pi" + tag], writes=["r2" + tag])
    S.dve(lambda e: e.scalar_tensor_tensor(out=r1[:, :T], in0=r2[:, :T], scalar=-C1, in1=pf[:, :T],
                                           op0=ALU.mult, op1=ALU.add), reads=["r2" + tag, "pf" + tag], writes=["r1" + tag])
    S.dve(lambda e: e.scalar_tensor_tensor(out=r1[:, :T], in0=r2[:, :T], scalar=-C2, in1=r1[:, :T],
                                           op0=ALU.mult, op1=ALU.add), reads=["r2" + tag, "r1" + tag], writes=["r1" + tag])

    def wrap(buf, key):
        S.dve(lambda e: e.tensor_scalar(out=r2[:, :T], in0=buf[:, :T], scalar1=PI, scalar2=-2.0 * PI,
                                        op0=ALU.is_gt, op1=ALU.mult), reads=[key], writes=["r2" + tag])
        S.dve(lambda e: e.tensor_tensor(out=buf[:, :T], in0=buf[:, :T], in1=r2[:, :T], op=ALU.add),
              reads=[key, "r2" + tag], writes=[key])
        S.dve(lambda e: e.tensor_scalar(out=r2[:, :T], in0=buf[:, :T], scalar1=-PI, scalar2=2.0 * PI,
                                        op0=ALU.is_lt, op1=ALU.mult), reads=[key], writes=["r2" + tag])
        S.dve(lambda e: e.tensor_tensor(out=buf[:, :T], in0=buf[:, :T], in1=r2[:, :T], op=ALU.add),
              reads=[key, "r2" + tag], writes=[key])
    wrap(r1, "r1" + tag)
    S.act(lambda e: e.activation(out=ST[:, :T], in_=r1[:, :T], func=AF.Sin, scale=C.sscale[:, 0:1]),
          reads=["r1" + tag, "sscale"], writes=["ST" + tag])
    S.dve(lambda e: e.tensor_scalar(out=pf[:, :T], in0=r1[:, :T], scalar1=PI / 2, scalar2=None, op0=ALU.add),
          reads=["r1" + tag], writes=["pf" + tag])
    wrap(pf, "pf" + tag)
    S.act(lambda e: e.activation(out=CT[:, :T], in_=pf[:, :T], func=AF.Sin), reads=["pf" + tag], writes=["CT" + tag])


def build_odd():
    nc = bass.Bass("TRN2", target_bir_lowering=False)
    with ExitStack() as es:
        C = Ctx(nc, es)
        S = C.S
        xT = C.dram_in("xT", [D, SEQ], F32)
        mod_in = C.dram_in("modin", [128, 24], F32)
        g_in = C.dram_in("g", [D], F32)
        wqkv_in = C.dram_in("w_qkv", [D, 1536], F32)
        wo_in = C.dram_in("w_o", [D, D], F32)
        sink_in = C.dram_in("sinkrep", [128, 16], F32)
        pos_in = C.dram_in("posrep", [128, SEQ], I32)
        ropec_in = C.dram_in("ropec", [128, 4], F32)
        mask_in = C.dram_in("masks", [128, 2, 256], BF16)
        fl_in = C.dram_in("flags", [128, 2], F32)
        out = C.dram_out("out", [D, OWN], F32)

        emit_consts(C)
        emit_adaln(C, mod_in, g_in)
        ropec = C.sb("ropec", [128, 4], F32)
        C.invf, C.pit, C.sbias, C.sscale = ropec[:, 0:1], ropec[:, 1:2], ropec[:, 2:3], ropec[:, 3:4]
        S.dma(SP, lambda e: e.dma_start(out=ropec[:], in_=ropec_in), writes=["invf"])
        for kname in ("pit", "sbias", "sscale"):
            S.lastw[kname] = S.lastw["invf"]
        flags = C.sb("flags", [128, 2], F32)
        masks = C.sb("masks", [128, 2, 256], BF16)
        esk = C.sb("esk", [128, 16], F32)
        ones64 = C.sb("ones64", [128, 64], BF16)
        S.dma(SP, lambda e: e.dma_start(out=flags[:], in_=fl_in), writes=["flags"])
        S.dma(SP, lambda e: e.dma_start(out=masks[:], in_=mask_in), writes=["masks"])
        S.dma(SP, lambda e: e.dma_start(out=esk[:], in_=sink_in), writes=["esk"])
        S.act(lambda e: e.activation(out=esk[:], in_=esk[:], func=AF.Exp), reads=["esk"], writes=["esk"])
        S.dve(lambda e: e.memset(ones64[:], 1.0), writes=["ones64"])
        medge = C.sb("medge", [128, 2, 256], BF16)
        S.dve(lambda e: e.tensor_scalar(out=medge[:, 0, :], in0=masks[:, 0, :], scalar1=flags[:, 0:1], scalar2=None,
                                        op0=ALU.mult), reads=["masks", "flags"], writes=["medge0"])
        S.dve(lambda e: e.tensor_scalar(out=medge[:, 1, :], in0=masks[:, 1, :], scalar1=flags[:, 1:2], scalar2=None,
                                        op0=ALU.mult), reads=["masks", "flags"], writes=["medge1"])
        kT = C.sb("kT", [128, 4, NKT * 128], BF16)
        V = C.sb("V", [128, NKT, 256], BF16)
        T = 512
        xs = C.sb("xs", [128, 8, T], F32)
        nb = {"sq": C.sb("sq", [128, 8, T], BF16), "rstd": C.sb("rstd", [128, T], F32),
              "tmp": C.sb("tmp", [128, 8, T], F32)}
        hT = C.sb("hT", [128, 8, T], BF16)
        rb = {"pi": C.sb("pi", [128, T], I32), "pf": C.sb("pf", [128, T], F32), "r1": C.sb("r1", [128, T], F32),
              "r2": C.sb("r2", [128, T], F32), "CT": C.sb("CT", [128, T], F32), "ST": C.sb("ST", [128, T], F32)}
        t1 = [C.sb("t1%d" % i, [128, T], F32) for i in range(2)]
        t2 = [C.sb("t2%d" % i, [128, T], F32) for i in range(2)]
        mk0 = C.mark()
        wk = C.sb("wk", [128, 8, 4, 128], BF16)
        wkp = C.sb("wkp", [128, 8, 4, 128], BF16)
        wv = C.sb("wv", [128, 8, 256], BF16)
        wsrc = wqkv_in.rearrange("(k p) n -> p k n", p=128)
        wkk = []
        wkpk = []
        for k in range(8):
            ksrc = wsrc[:, k, 1024:1280].rearrange("p (j two i) -> p j two i", j=4, two=2)
            for dup in range(2):
                S.dma(POOL, lambda e, dup=dup, k=k: e.dma_start(
                    out=wk[:, k, :, dup * 64:(dup + 1) * 64],
                    in_=wsrc[:, k, 1024:1280].rearrange("p (j c) -> p j c", j=4)), writes=[("wk", dup, k)], sem_key="wkld")
                wkk.append(("wk", dup, k))
                for two in range(2):
                    S.dma(POOL, lambda e, dup=dup, two=two, k=k, ksrc=ksrc: e.dma_start(
                        out=wkp[:, k, :, dup * 64 + two * 32:dup * 64 + two * 32 + 32], in_=ksrc[:, :, 1 - two, :]),
                        writes=[("wkp", dup, two, k)], sem_key="wkld")
                    wkpk.append(("wkp", dup, two, k))
        S.dma(POOL, lambda e: e.dma_start(out=wv[:], in_=wsrc[:, :, 1280:1536]), writes=["wv"])
        xv = xT.rearrange("(k p) t -> p k t", p=128)
        hk = [("hT", k) for k in range(8)]
        blocks = [(SEQ - 128, 128, 0)] + [(j * T, T, 1 + 4 * j) for j in range(OWN // T)] + [(OWN, 128, NKT - 1)]
        for (c0, TT, kt0) in blocks:
            S.dma(SP, lambda e, c0=c0, TT=TT: e.dma_start(out=xs[:, :, :TT], in_=xv[:, :, c0:c0 + TT]), writes=["xs"])
            emit_norm(C, xs, "xs", hT, hk, TT, 0, nb)
            emit_rope_tables(C, pos_in, c0, TT, rb, "")
            for j in range(4):
                par = j % 2
                pa, pb = 1 + par, 3 + par

                def mma(e, j=j, pa=pa, TT=TT):
                    for k in range(8):
                        r = e.matmul(C.ps[pa][:, :TT], lhsT=wk[:, k, j, :], rhs=hT[:, k, :TT], start=(k == 0), stop=(k == 7))
                    return r

                def mmb(e, j=j, pb=pb, TT=TT):
                    for k in range(8):
                        r = e.matmul(C.ps[pb][:, :TT], lhsT=wkp[:, k, j, :], rhs=hT[:, k, :TT], start=(k == 0), stop=(k == 7))
                    return r
                S.pe(mma, reads=wkk + hk, writes=[("ps", pa)])
                S.pe(mmb, reads=wkpk + hk, writes=[("ps", pb)])
                S.dve(lambda e, pa=pa, par=par, TT=TT: e.tensor_tensor(out=t1[par][:, :TT], in0=C.ps[pa][:, :TT],
                                                                       in1=rb["CT"][:, :TT], op=ALU.mult),
                      reads=[("ps", pa), "CT"], writes=[("t1", par)])
                S.dve(lambda e, pb=pb, par=par, TT=TT: e.tensor_tensor(out=t2[par][:, :TT], in0=C.ps[pb][:, :TT],
                                                                       in1=rb["ST"][:, :TT], op=ALU.mult),
                      reads=[("ps", pb), "ST"], writes=[("t2", par)])
                S.pool(lambda e, j=j, par=par, TT=TT, kt0=kt0: e.tensor_tensor(
                    out=kT[:, j, kt0 * 128:kt0 * 128 + TT], in0=t1[par][:, :TT], in1=t2[par][:, :TT], op=ALU.add),
                    reads=[("t1", par), ("t2", par)], writes=[("kT", j, kt0 + q_) for q_ in range(TT // 128)])
            for tt in range(TT // 128):
                pv = 5 + (tt % 2)

                def mmv(e, tt=tt, pv=pv):
                    for k in range(8):
                        r = e.matmul(C.ps[pv][:, 0:256], lhsT=hT[:, k, tt * 128:(tt + 1) * 128], rhs=wv[:, k, :],
                                     start=(k == 0), stop=(k == 7))
                    return r
                S.pe(mmv, reads=["wv"] + hk, writes=[("ps", pv)])
                S.act(lambda e, tt=tt, pv=pv, kt0=kt0: e.activation(out=V[:, kt0 + tt, :], in_=C.ps[pv][:, 0:256], func=AF.Copy),
                      reads=[("ps", pv)], writes=[("V", kt0 + tt)])
        import os
        dbg = os.environ.get("ODD_DBG", "")
        if dbg == "A":
            S.emit()
            return nc
        S.barrier()
        if dbg == "AB":
            S.emit()
            return nc
        C.reset(mk0)
        wq = C.sb("wq", [128, 8, D], BF16)
        wqp = C.sb("wqp", [128, 8, D], BF16)
        wo = C.sb("wo", [128, 8, D], BF16)
        qT = C.sb("qT", [128, 8, T], BF16)
        oT = C.sb("oT", [128, 8, T], BF16)
        NEB = 6
        Eb = [C.sb("E%d" % i, [128, 256], BF16) for i in range(NEB)]
        rc = [C.sb("rc%d" % i, [128, 256], F32) for i in range(2)]
        S.dma(POOL, lambda e: e.dma_start(out=wq[:], in_=wsrc[:, :, 0:1024]), writes=["wq"])
        wqpk = []
        for k in range(8):
            qsrc = wsrc[:, k, 0:1024].rearrange("p (h two i) -> p h two i", h=16, two=2)
            for two in range(2):
                S.dma(POOL, lambda e, two=two, k=k, qsrc=qsrc: e.dma_start(
                    out=wqp[:, k, :].rearrange("p (h two i) -> p h two i", h=16, two=2)[:, :, two, :],
                    in_=qsrc[:, :, 1 - two, :]), writes=[("wqp", two, k)], sem_key="wqld")
                wqpk.append(("wqp", two, k))
        S.dma(POOL, lambda e: e.dma_start(out=wo[:], in_=wo_in.rearrange("(k p) n -> p k n", p=128)), writes=["wo"])
        ov = out.rearrange("(k p) t -> p k t", p=128)
        ecnt = 0
        gcnt = 0
        for jb in range(OWN // T):
            c0 = jb * T
            S.dma(SP, lambda e, c0=c0: e.dma_start(out=xs[:], in_=xv[:, :, c0:c0 + T]), writes=["xs"])
            emit_norm(C, xs, "xs", hT, hk, T, 0, nb)
            emit_rope_tables(C, pos_in, c0, T, rb, "")
            for ch in range(8):
                par = ch % 2
                pa, pb = 1 + par, 3 + par

                def mma(e, ch=ch, pa=pa):
                    for k in range(8):
                        r = e.matmul(C.ps[pa][:], lhsT=wq[:, k, ch * 128:(ch + 1) * 128], rhs=hT[:, k, :],
                                     start=(k == 0), stop=(k == 7))
                    return r

                def mmb(e, ch=ch, pb=pb):
                    for k in range(8):
                        r = e.matmul(C.ps[pb][:], lhsT=wqp[:, k, ch * 128:(ch + 1) * 128], rhs=hT[:, k, :],
                                     start=(k == 0), stop=(k == 7))
                    return r
                S.pe(mma, reads=["wq"] + hk, writes=[("ps", pa)])
                S.pe(mmb, reads=wqpk + hk, writes=[("ps", pb)])
                S.dve(lambda e, pa=pa, par=par: e.tensor_tensor(out=t1[par][:], in0=C.ps[pa][:], in1=rb["CT"][:], op=ALU.mult),
                      reads=[("ps", pa), "CT"], writes=[("t1", par)])
                S.dve(lambda e, pb=pb, par=par: e.tensor_tensor(out=t2[par][:], in0=C.ps[pb][:], in1=rb["ST"][:], op=ALU.mult),
                      reads=[("ps", pb), "ST"], writes=[("t2", par)])
                S.pool(lambda e, ch=ch, par=par: e.tensor_tensor(out=qT[:, ch, :], in0=t1[par][:], in1=t2[par][:], op=ALU.add),
                       reads=[("t1", par), ("t2", par)], writes=[("qT", ch)])
            if dbg == "B1":
                S.dma(SP, lambda e, c0=c0: e.dma_start(out=ov[:, :, c0:c0 + T], in_=xs[:]),
                      reads=["xs"] + [("qT", ch) for ch in range(8)], writes=["OUT"], sem_key="xost")
                continue
            for qb in range(4):
                n = jb * 4 + qb
                for kv in range(4):
                    for half in range(2):
                        p0 = half * 64
                        gi = gcnt
                        gcnt += 1
                        pnum = 5 + (gi % 2)
                        ba = 1 + 2 * (gi % 2)
                        ebs = []
                        for dl in range(3):
                            ebs.append(ecnt % NEB)
                            ecnt += 1

                        def mmsc(e, kv=kv, p0=p0, n=n, qb=qb, ba=ba):
                            for dl in range(3):
                                kt = n + dl
                                dst = C.ps[ba][:, dl * 256:(dl + 1) * 256] if dl < 2 else C.ps[ba + 1][:, 0:256]
                                r = e.matmul(dst, lhsT=kT[p0:p0 + 64, kv, kt * 128:(kt + 1) * 128],
                                             rhs=qT[p0:p0 + 64, 2 * kv:2 * kv + 2, qb * 128:(qb + 1) * 128],
                                             start=True, stop=True)
                            return r
                        S.pe(mmsc, reads=[("kT", kv, n + dl) for dl in range(3)] + [("qT", 2 * kv), ("qT", 2 * kv + 1)],
                             writes=[("ps", ba), ("ps", ba + 1)])
                        for dl in range(3):
                            eb = ebs[dl]
                            src = C.ps[ba][:, dl * 256:(dl + 1) * 256] if dl < 2 else C.ps[ba + 1][:, 0:256]
                            S.act(lambda e, src=src, eb=eb: e.activation(out=Eb[eb][:], in_=src, func=AF.Exp, scale=0.125),
                                  reads=[("ps", ba if dl < 2 else ba + 1)], writes=[("E", eb)])
                            if dl != 1:
                                mi = 0 if dl == 0 else 1
                                edge = (n == 0 and dl == 0) or (n == OWN // 128 - 1 and dl == 2)
                                msrc = medge if edge else masks
                                S.pool(lambda e, eb=eb, mi=mi, msrc=msrc: e.tensor_tensor(
                                    out=Eb[eb][:], in0=Eb[eb][:], in1=msrc[:, mi, :], op=ALU.mult),
                                    reads=[("E", eb), "masks", "medge0", "medge1"], writes=[("E", eb)])
                        if dbg == "B2":
                            continue

                        def mmpv(e, kv=kv, p0=p0, n=n, ebs=ebs, pnum=pnum):
                            for dl in range(3):
                                e.matmul(C.ps[pnum][p0:p0 + 64, 0:256], lhsT=V[:, n + dl, kv * 64:(kv + 1) * 64],
                                         rhs=Eb[ebs[dl]][:], start=(dl == 0), stop=(dl == 2))
                            for dl in range(3):
                                r = e.matmul(C.ps[pnum][p0:p0 + 64, 256:512], lhsT=ones64[:], rhs=Eb[ebs[dl]][:],
                                             start=(dl == 0), stop=(dl == 2))
                            return r
                        S.pe(mmpv, reads=[("V", n + dl) for dl in range(3)] + [("E", eb) for eb in ebs] + ["ones64"],
                             writes=[("ps", pnum)])
                        r_ = rc[gi % 2]
                        for hh in range(2):
                            head = 4 * kv + 2 * hh + half
                            S.dve(lambda e, p0=p0, pnum=pnum, hh=hh, head=head, r_=r_: e.tensor_scalar(
                                out=r_[p0:p0 + 64, hh * 128:(hh + 1) * 128], in0=C.ps[pnum][p0:p0 + 64, 256 + hh * 128:256 + (hh + 1) * 128],
                                scalar1=esk[p0:p0 + 64, head:head + 1], scalar2=None, op0=ALU.add),
                                reads=[("ps", pnum), "esk"], writes=[("rc", gi % 2, half)])
                        S.dve(lambda e, p0=p0, r_=r_: e.reciprocal(out=r_[p0:p0 + 64, :], in_=r_[p0:p0 + 64, :]),
                              reads=[("rc", gi % 2, half)], writes=[("rc", gi % 2, half)])
                        S.dve(lambda e, p0=p0, pnum=pnum, kv=kv, qb=qb, r_=r_: e.tensor_tensor(
                            out=oT[p0:p0 + 64, 2 * kv:2 * kv + 2, qb * 128:(qb + 1) * 128],
                            in0=C.ps[pnum][p0:p0 + 64, 0:256].rearrange("p (a b) -> p a b", a=2),
                            in1=r_[p0:p0 + 64, :].rearrange("p (a b) -> p a b", a=2), op=ALU.mult),
                            reads=[("ps", pnum), ("rc", gi % 2, half)],
                            writes=[("oT", 2 * kv, qb, half), ("oT", 2 * kv + 1, qb, half)])
            if dbg == "B2":
                S.dma(SP, lambda e, c0=c0: e.dma_start(out=ov[:, :, c0:c0 + T], in_=xs[:]),
                      reads=["xs"] + [("E", eb) for eb in range(NEB)], writes=["OUT"], sem_key="xost")
                continue
            for dch in range(8):
                pb = 7

                def mmo(e, dch=dch):
                    for c in range(8):
                        r = e.matmul(C.ps[7][:], lhsT=wo[:, c, dch * 128:(dch + 1) * 128], rhs=oT[:, c, :],
                                     start=(c == 0), stop=(c == 7))
                    return r
                S.pe(mmo, reads=["wo"] + [("oT", c, qb, half) for c in range(8) for qb in range(4) for half in range(2)],
                     writes=[("ps", 7)])
                S.dve(lambda e, dch=dch: e.scalar_tensor_tensor(
                    out=xs[:, dch, :], in0=C.ps[7][:], scalar=C.mod[:, 16 + dch:17 + dch], in1=xs[:, dch, :],
                    op0=ALU.mult, op1=ALU.add), reads=[("ps", 7), "xs", "mod"], writes=["xs"])
            S.dma(SP, lambda e, c0=c0: e.dma_start(out=ov[:, :, c0:c0 + T], in_=xs[:]),
                  reads=["xs"], writes=["OUT"], sem_key="xs")
        S.emit()
    return nc


_PROGS = {}


def _prog(name, builder, *args):
    key = (name,) + args
    if key not in _PROGS:
        _PROGS[key] = builder(*args)
    return _PROGS[key]


def _moe_consts():
    p = np.arange(128)
    blk = (p[:, None] // 8 == p[None, :] // 8).astype(np.float32)
    sel = (p[:, None] == 8 * np.arange(NE)[None, :]).astype(np.float32)
    selE = np.zeros((NE, NE, 128), np.float32)
    for e in range(NE):
        selE[e, e, :] = 1.0
    return {"blkones": blk, "sel": sel, "selE": selE}


def _perm_xt(XT, b, h):
    own = XT[b][:, h * OWN:(h + 1) * OWN]
    oth = XT[b][:, (1 - h) * OWN:(2 - h) * OWN]
    return np.ascontiguousarray(np.concatenate([own, oth], axis=1))


def run_ada(inp):
    nc = _prog("ada", build_ada)
    in_maps = []
    for core in range(8):
        l, j = core // 2, core % 2
        in_maps.append({"c": np.ascontiguousarray(inp["c"]), "adaw": np.ascontiguousarray(inp["ada_w"][l, j]),
                        "adab": np.ascontiguousarray(inp["ada_b"][l, j])})
    res = run_bass_kernel_spmd(nc, in_maps, core_ids=list(range(8)))
    mods = {}
    for core in range(8):
        l, j = core // 2, core % 2
        m = res.results[core]["mod"]
        for b in range(4):
            mods[(l, j, b)] = np.ascontiguousarray(m[:, :, b])
    return mods


def run_moe(XT, l, inp, mods, final):
    consts = _moe_consts()
    common = []
    for core in range(8):
        b = core // 2
        common.append({"modin": mods[(l, 1, b)], "g": np.ascontiguousarray(inp["ffn_norm_g"][l])})
    nc1 = _prog("route", build_route)
    in_maps = []
    for core in range(8):
        b, h = core // 2, core % 2
        m = dict(common[core])
        m.update({"xT": _perm_xt(XT, b, h), "rw": np.ascontiguousarray(inp["router_w"][l]),
                  "rb": np.ascontiguousarray(inp["router_b"][l]),
                  "blkones": consts["blkones"], "sel": consts["sel"]})
        in_maps.append(m)
    res1 = run_bass_kernel_spmd(nc1, in_maps, core_ids=list(range(8)))
    nc2 = _prog("experts", build_experts, final)
    in_maps = []
    for core in range(8):
        b, h = core // 2, core % 2
        m = dict(common[core])
        m.update({"xT": np.ascontiguousarray(XT[b][:, h * OWN:(h + 1) * OWN]), "wgt": res1.results[core]["wgt"],
                  "wg": np.ascontiguousarray(inp["moe_w_gate"][l]), "wu": np.ascontiguousarray(inp["moe_w_up"][l]),
                  "wd": np.ascontiguousarray(inp["moe_w_down"][l]), "selE": consts["selE"]})
        if final:
            m["fg"] = np.ascontiguousarray(inp["final_norm_g"])
        in_maps.append(m)
    res = run_bass_kernel_spmd(nc2, in_maps, core_ids=list(range(8)))
    out = np.empty_like(XT)
    for core in range(8):
        b, h = core // 2, core % 2
        out[b][:, h * OWN:(h + 1) * OWN] = res.results[core]["out"]
    return out


def _dft_consts():
    import ml_dtypes
    outc = {}
    sp = np.arange(SEQ, dtype=np.int64)
    kp = np.arange(OWN, dtype=np.int64)
    for h in range(2):
        s_nat = (sp + OWN * h) % SEQ
        k_nat = kp + OWN * h
        m = (s_nat[:, None] * k_nat[None, :]) % SEQ
        ang = (2.0 * np.pi / SEQ) * m.astype(np.float64)
        outc[h] = (np.cos(ang).astype(np.float32).astype(ml_dtypes.bfloat16),
                   (-np.sin(ang)).astype(np.float32).astype(ml_dtypes.bfloat16))
    c = np.arange(128, dtype=np.int64)
    a128 = (2.0 * np.pi / 128) * ((c[:, None] * c[None, :]) % 128).astype(np.float64)
    cs128 = np.concatenate([np.cos(a128), np.sin(a128)], axis=1).astype(np.float32).astype(ml_dtypes.bfloat16)
    return outc, cs128


_CONSTS = {}


def run_even(XT, l, inp, mods):
    i = l // 2
    nc = _prog("even", build_even)
    if "dft" not in _CONSTS:
        _CONSTS["dft"] = _dft_consts()
    dft, cs128 = _CONSTS["dft"]
    ident = np.eye(128, dtype=np.float32)
    in_maps = []
    for core in range(8):
        b, h = core // 2, core % 2
        flags = np.zeros((128, 2), np.float32)
        flags[:, 0] = 1.0 if h == 1 else 0.0
        flags[:, 1] = 1.0 if h == 0 else 0.0
        in_maps.append({
            "xT": _perm_xt(XT, b, h), "modin": mods[(l, 0, b)], "g": np.ascontiguousarray(inp["mix_norm_g"][l]),
            "w_in": np.ascontiguousarray(inp["fc_w_in"][i]), "w_out": np.ascontiguousarray(inp["fc_w_out"][i]),
            "conv_w": np.ascontiguousarray(inp["conv_w"][i]), "conv_b": np.ascontiguousarray(inp["conv_b"][i]),
            "ln_g": np.ascontiguousarray(inp["conv_ln_g"][i]), "ln_b": np.ascontiguousarray(inp["conv_ln_b"][i]),
            "dftC": dft[h][0], "dftNS": dft[h][1], "cs128": cs128, "flags": flags, "ident": ident})
    res = run_bass_kernel_spmd(nc, in_maps, core_ids=list(range(8)))
    out = np.empty_like(XT)
    for core in range(8):
        b, h = core // 2, core % 2
        out[b][:, h * OWN:(h + 1) * OWN] = res.results[core]["out"]
    return out


def run_odd(XT, l, inp, mods):
    import ml_dtypes
    i = l // 2
    nc = _prog("odd", build_odd)
    p = np.arange(128)
    inv = (10000.0 ** (-np.arange(0, 64, 2, dtype=np.float32) / 64)).astype(np.float32)
    ropec = np.zeros((128, 4), np.float32)
    ropec[:, 0] = inv[p % 32]
    ropec[:, 1] = np.float32(np.pi)
    second = (p % 64) >= 32
    ropec[:, 3] = np.where(second, 1.0, -1.0)
    jj = np.arange(128)[:, None]
    ii = np.arange(128)[None, :]
    ml = (jj >= ii).astype(np.float32)
    mr = (jj <= ii).astype(np.float32)
    masks = np.stack([np.concatenate([ml, ml], 1), np.concatenate([mr, mr], 1)], axis=1).astype(ml_dtypes.bfloat16)
    in_maps = []
    for core in range(8):
        b, h = core // 2, core % 2
        flags = np.zeros((128, 2), np.float32)
        flags[:, 0] = 1.0 if h == 1 else 0.0
        flags[:, 1] = 1.0 if h == 0 else 0.0
        pos = inp["positions"][b]
        posp = np.concatenate([pos[h * OWN:(h + 1) * OWN], pos[(1 - h) * OWN:(2 - h) * OWN]])
        in_maps.append({
            "xT": _perm_xt(XT, b, h), "modin": mods[(l, 0, b)], "g": np.ascontiguousarray(inp["mix_norm_g"][l]),
            "w_qkv": np.ascontiguousarray(inp["attn_w_qkv"][i]), "w_o": np.ascontiguousarray(inp["attn_w_out"][i]),
            "sinkrep": np.ascontiguousarray(np.broadcast_to(inp["attn_sink"][i][None, :], (128, 16))),
            "posrep": np.ascontiguousarray(np.broadcast_to(posp[None, :], (128, SEQ))).astype(np.int32),
            "ropec": ropec, "masks": masks, "flags": flags})
    res = run_bass_kernel_spmd(nc, in_maps, core_ids=list(range(8)))
    out = np.empty_like(XT)
    for core in range(8):
        b, h = core // 2, core % 2
        out[b][:, h * OWN:(h + 1) * OWN] = res.results[core]["out"]
    return out


def kernel(**inp):
    inp = {k: np.asarray(v) for k, v in inp.items()}
    mods = run_ada(inp)
    XT = np.ascontiguousarray(inp["x"].transpose(0, 2, 1))
    for l in range(4):
        if l % 2 == 0:
            XT = run_even(XT, l, inp, mods)
        else:
            XT = run_odd(XT, l, inp, mods)
        XT = run_moe4(XT, l, inp, mods, final=(l == 3))
    return np.ascontiguousarray(XT.transpose(0, 2, 1))


def run_moe4(XT, l, inp, mods, final):
    consts = _moe_consts()
    nc = _prog("moe4", build_moe4, final)
    in_maps = []
    for b in range(4):
        m = {"modin": mods[(l, 1, b)], "g": np.ascontiguousarray(inp["ffn_norm_g"][l]),
             "xT": np.ascontiguousarray(XT[b]), "rw": np.ascontiguousarray(inp["router_w"][l]),
             "rb": np.ascontiguousarray(inp["router_b"][l]), "blkones": consts["blkones"], "sel": consts["sel"],
             "wg": np.ascontiguousarray(inp["moe_w_gate"][l]), "wu": np.ascontiguousarray(inp["moe_w_up"][l]),
             "wd": np.ascontiguousarray(inp["moe_w_down"][l]), "selE": consts["selE"]}
        if final:
            m["fg"] = np.ascontiguousarray(inp["final_norm_g"])
        in_maps.append(m)
    res = run_bass_kernel_spmd(nc, in_maps, core_ids=list(range(4)))
    return np.stack([res.results[b]["out"] for b in range(4)], axis=0)
```
